# Optimizing a Trainium2 kernel written in Bass

```python
import functools
import jax, jax.numpy as jnp
from jax import lax
import numpy as np

D_MODEL = 1024
BATCH = 8
SEQ = 2048
DEPTH = 1

DN_HEADS = 8
DN_HEAD_DIM = 128
DN_DIM = DN_HEADS * DN_HEAD_DIM
DN_CHUNK = 64
CONV_WIDTH = 4
SGU_GROUPS = 8
SGU_GROUP_DIM = 128
SGU_DIM = SGU_GROUPS * SGU_GROUP_DIM
SGU_CHUNK = 128
N_GROUPS = 4
EXPERTS_PER_GROUP = 4
N_EXPERTS = N_GROUPS * EXPERTS_PER_GROUP
TOP_K_IN_GROUP = 2
EXPERT_FF = 256
DEEPNORM_ALPHA = (2.0 * DEPTH) ** 0.25
DEEPNORM_BETA = (8.0 * DEPTH) ** -0.25
LN_EPS = 1e-5
NORM_EPS = 1e-6
PROJ_SIZES = (3 * DN_DIM, DN_DIM, DN_HEADS, DN_HEADS, 2 * SGU_DIM, D_MODEL, D_MODEL)
PROJ_DIM = sum(PROJ_SIZES)

kernel_name = "hybrid_deltanet_sgu_hmoe_deepnorm"


def layer_norm(x, g, b):
    xf = x.astype(jnp.float32)
    xc = xf - jnp.mean(xf, axis=-1, keepdims=True)
    var = jnp.mean(xc * xc, axis=-1, keepdims=True)
    return (xc * lax.rsqrt(var + LN_EPS) * g + b).astype(x.dtype)


def l2_normalize(x):
    return x * lax.rsqrt(jnp.sum(x * x, axis=-1, keepdims=True) + NORM_EPS)


def causal_depthwise_conv(x, w):
    k = w.shape[0]
    return lax.conv_general_dilated(
        x, w[:, None, :], window_strides=(1,), padding=[(k - 1, 0)],
        dimension_numbers=("NWC", "WIO", "NWC"), feature_group_count=x.shape[-1])


def gated_delta_rule(q, k, v, g, beta):
    bsz, seq, heads, dk = q.shape
    dv = v.shape[-1]
    n = seq // DN_CHUNK

    def to_chunks(t):
        return jnp.swapaxes(t.reshape(bsz, n, DN_CHUNK, heads, *t.shape[3:]), 2, 3)

    q, k, v, g, beta = (to_chunks(t) for t in (q, k, v, g, beta))
    g_cum = jnp.cumsum(g, axis=-1)
    idx = jnp.arange(DN_CHUNK)
    causal = idx[:, None] >= idx[None, :]
    strict = idx[:, None] > idx[None, :]
    diff = g_cum[..., :, None] - g_cum[..., None, :]
    decay = jnp.where(causal, jnp.exp(jnp.where(causal, diff, 0.0)), 0.0)
    k_beta = k * beta[..., None]
    v_beta = v * beta[..., None]
    a_mat = jnp.where(strict, jnp.einsum("bnhtk,bnhsk->bnhts", k_beta, k) * decay, 0.0) \
        + jnp.eye(DN_CHUNK, dtype=q.dtype)
    solve = functools.partial(lax.linalg.triangular_solve, left_side=True, lower=True,
                              unit_diagonal=True)
    u = solve(a_mat, v_beta)
    w = solve(a_mat, k_beta * jnp.exp(g_cum)[..., None])
    qk = jnp.einsum("bnhtk,bnhsk->bnhts", q, k) * decay
    q_dec = q * jnp.exp(g_cum)[..., None]
    k_dec = k * jnp.exp(g_cum[..., -1:] - g_cum)[..., None]
    g_last = jnp.exp(g_cum[..., -1])

    def step(state, inp):
        qk_c, u_c, w_c, q_c, k_c, gl_c = inp
        v_new = u_c - jnp.einsum("bhtk,bhkv->bhtv", w_c, state)
        out = jnp.einsum("bhtk,bhkv->bhtv", q_c, state) + jnp.einsum("bhts,bhsv->bhtv", qk_c, v_new)
        state = state * gl_c[..., None, None] + jnp.einsum("bhsk,bhsv->bhkv", k_c, v_new)
        return state, out

    xs = tuple(jnp.moveaxis(t, 1, 0) for t in (qk, u, w, q_dec, k_dec, g_last))
    state0 = jnp.zeros((bsz, heads, dk, dv), jnp.float32)
    _, out = lax.scan(step, state0, xs)
    return jnp.transpose(out, (1, 0, 3, 2, 4)).reshape(bsz, seq, heads, dv)


def spatial_gating(uv, ln_g, ln_b, spatial_w, spatial_b, w_sgu_out):
    bsz, seq, _ = uv.shape
    n = seq // SGU_CHUNK
    u, v = jnp.split(jax.nn.gelu(uv, approximate=False), 2, axis=-1)
    v = layer_norm(v, ln_g, ln_b)
    v = v.reshape(bsz, n, SGU_CHUNK, SGU_GROUPS, SGU_GROUP_DIM)
    idx = jnp.arange(SGU_CHUNK)
    causal = idx[:, None] >= idx[None, :]
    w_causal = jnp.where(causal, spatial_w, 0)
    v = jnp.einsum("gts,bnsgc->bntgc", w_causal, v) + spatial_b.T[:, :, None]
    gated = u * v.reshape(bsz, seq, SGU_DIM)
    return jnp.einsum("blc,cd->bld", gated, w_sgu_out)


def hybrid_mixer(x, w_in, conv_w, a_log, dt_bias, dn_norm_g, w_dn_out,
                 sgu_ln_g, sgu_ln_b, spatial_w, spatial_b, w_sgu_out, w_out):
    bsz, seq, _ = x.shape
    f32 = jnp.float32
    proj = jnp.einsum("bld,dp->blp", x, w_in)
    offsets = np.cumsum(PROJ_SIZES)[:-1].tolist()
    qkv, z, a, b, uv, gate_dn, gate_sgu = jnp.split(proj, offsets, axis=-1)

    qkv = jax.nn.silu(causal_depthwise_conv(qkv, conv_w)).astype(f32)
    q, k, v = jnp.split(qkv, 3, axis=-1)
    to_heads = lambda t: t.reshape(bsz, seq, DN_HEADS, DN_HEAD_DIM)
    q = l2_normalize(to_heads(q)) * (DN_HEAD_DIM ** -0.5)
    k = l2_normalize(to_heads(k))
    v = to_heads(v)
    beta = jax.nn.sigmoid(b.astype(f32))
    g = -jnp.exp(a_log.astype(f32)) * jax.nn.softplus(a.astype(f32) + dt_bias.astype(f32))
    o = gated_delta_rule(q, k, v, g, beta)
    o = o * lax.rsqrt(jnp.mean(o * o, axis=-1, keepdims=True) + NORM_EPS) * dn_norm_g.astype(f32)
    o = o * jax.nn.silu(to_heads(z).astype(f32))
    y_dn = jnp.einsum("blc,cd->bld", o.reshape(bsz, seq, DN_DIM).astype(x.dtype), w_dn_out)

    y_sgu = spatial_gating(uv, sgu_ln_g, sgu_ln_b, spatial_w, spatial_b, w_sgu_out)

    y = jax.nn.sigmoid(gate_dn) * y_dn + jax.nn.sigmoid(gate_sgu) * y_sgu
    return jnp.einsum("bld,de->ble", y, w_out)


def hierarchical_moe(x, router_group_w, router_group_b, router_expert_w, router_expert_b,
                     expert_w_gate, expert_w_up, expert_w_down):
    bsz, seq, d = x.shape
    f32 = jnp.float32
    t = x.reshape(bsz * seq, d)
    n_tok = t.shape[0]
    group_logits = (t @ router_group_w + router_group_b).astype(f32)
    group_p, group_idx = lax.top_k(jax.nn.softmax(group_logits, axis=-1), 1)
    group_p, group_idx = group_p[:, 0], group_idx[:, 0]
    expert_logits = (t @ router_expert_w + router_expert_b).astype(f32)
    expert_logits = expert_logits.reshape(n_tok, N_GROUPS, EXPERTS_PER_GROUP)
    in_group = expert_logits[jnp.arange(n_tok), group_idx]
    exp_p, exp_idx = lax.top_k(jax.nn.softmax(in_group, axis=-1), TOP_K_IN_GROUP)
    exp_p = exp_p / jnp.sum(exp_p, axis=-1, keepdims=True)
    gate = group_p[:, None] * exp_p
    expert_id = group_idx[:, None] * EXPERTS_PER_GROUP + exp_idx
    combine = jnp.sum(jax.nn.one_hot(expert_id, N_EXPERTS, dtype=f32) * gate[..., None], axis=1)
    h = jax.nn.silu(jnp.einsum("td,edf->tef", t, expert_w_gate)) \
        * jnp.einsum("td,edf->tef", t, expert_w_up)
    h = h * combine[:, :, None].astype(h.dtype)
    y = jnp.einsum("tef,efd->td", h, expert_w_down)
    return y.reshape(bsz, seq, d)


def setup_inputs(seed: int = 0) -> dict:
    key = jax.random.key(seed)
    ks = jax.random.split(key, 24)
    f32 = jnp.float32
    nrm = lambda k, shape, scale: jax.random.normal(k, shape, f32) * scale
    dt = jnp.exp(jax.random.uniform(ks[4], (DEPTH, DN_HEADS), f32,
                                    minval=float(np.log(1e-3)), maxval=float(np.log(1e-1))))
    return {
        "x": nrm(ks[0], (BATCH, SEQ, D_MODEL), 1.0),
        "w_in": nrm(ks[1], (DEPTH, D_MODEL, PROJ_DIM), D_MODEL ** -0.5),
        "conv_w": nrm(ks[2], (DEPTH, CONV_WIDTH, 3 * DN_DIM), CONV_WIDTH ** -0.5),
        "a_log": jnp.log(jax.random.uniform(ks[3], (DEPTH, DN_HEADS), f32, minval=1.0, maxval=16.0)),
        "dt_bias": dt + jnp.log(-jnp.expm1(-dt)),
        "dn_norm_g": 1.0 + nrm(ks[5], (DEPTH, DN_HEAD_DIM), 0.1),
        "w_dn_out": nrm(ks[6], (DEPTH, DN_DIM, D_MODEL), DN_DIM ** -0.5),
        "sgu_ln_g": 1.0 + nrm(ks[7], (DEPTH, SGU_DIM), 0.1),
        "sgu_ln_b": nrm(ks[8], (DEPTH, SGU_DIM), 0.02),
        "spatial_w": nrm(ks[9], (DEPTH, SGU_GROUPS, SGU_CHUNK, SGU_CHUNK), 0.05),
        "spatial_b": 1.0 + nrm(ks[10], (DEPTH, SGU_GROUPS, SGU_CHUNK), 0.1),
        "w_sgu_out": nrm(ks[11], (DEPTH, SGU_DIM, D_MODEL), SGU_DIM ** -0.5),
        "w_out": nrm(ks[12], (DEPTH, D_MODEL, D_MODEL), D_MODEL ** -0.5 * DEEPNORM_BETA),
        "ln1_g": 1.0 + nrm(ks[13], (DEPTH, D_MODEL), 0.1),
        "ln1_b": nrm(ks[14], (DEPTH, D_MODEL), 0.02),
        "router_group_w": nrm(ks[15], (DEPTH, D_MODEL, N_GROUPS), D_MODEL ** -0.5),
        "router_group_b": nrm(ks[16], (DEPTH, N_GROUPS), 0.01),
        "router_expert_w": nrm(ks[17], (DEPTH, D_MODEL, N_EXPERTS), D_MODEL ** -0.5),
        "router_expert_b": nrm(ks[18], (DEPTH, N_EXPERTS), 0.01),
        "expert_w_gate": nrm(ks[19], (DEPTH, N_EXPERTS, D_MODEL, EXPERT_FF), D_MODEL ** -0.5),
        "expert_w_up": nrm(ks[20], (DEPTH, N_EXPERTS, D_MODEL, EXPERT_FF), D_MODEL ** -0.5),
        "expert_w_down": nrm(ks[21], (DEPTH, N_EXPERTS, EXPERT_FF, D_MODEL), EXPERT_FF ** -0.5 * DEEPNORM_BETA),
        "ln2_g": 1.0 + nrm(ks[22], (DEPTH, D_MODEL), 0.1),
        "ln2_b": nrm(ks[23], (DEPTH, D_MODEL), 0.02),
    }


def reference(x, w_in, conv_w, a_log, dt_bias, dn_norm_g, w_dn_out, sgu_ln_g, sgu_ln_b,
              spatial_w, spatial_b, w_sgu_out, w_out, ln1_g, ln1_b,
              router_group_w, router_group_b, router_expert_w, router_expert_b,
              expert_w_gate, expert_w_up, expert_w_down, ln2_g, ln2_b):
    h = x
    for l in range(DEPTH):
        mix = hybrid_mixer(h, w_in[l], conv_w[l], a_log[l], dt_bias[l], dn_norm_g[l], w_dn_out[l],
                           sgu_ln_g[l], sgu_ln_b[l], spatial_w[l], spatial_b[l], w_sgu_out[l], w_out[l])
        h = layer_norm(DEEPNORM_ALPHA * h + mix, ln1_g[l], ln1_b[l])
        ffn = hierarchical_moe(h, router_group_w[l], router_group_b[l], router_expert_w[l],
                               router_expert_b[l], expert_w_gate[l], expert_w_up[l], expert_w_down[l])
        h = layer_norm(DEEPNORM_ALPHA * h + ffn, ln2_g[l], ln2_b[l])
    return h
```

```python
import contextlib
import os
import numpy as np
import concourse.bass as bass
import concourse.mybir as mybir
from concourse.bass_utils import run_bass_kernel_spmd

F32 = mybir.dt.float32
BF16 = mybir.dt.bfloat16
AF = mybir.ActivationFunctionType
ALU = mybir.AluOpType
AX = mybir.AxisListType

PE, DVE, ACT, POOL, SP = "tensor", "vector", "scalar", "gpsimd", "sync"
ENGS = (PE, DVE, ACT, POOL, SP)
NDMASEM = 8
ATTACH = os.environ.get('KATTACH', '1') == '1'
WAR_SKIP = {'0': (), 'DA': (DVE, ACT), 'DAP': (DVE, ACT, POOL)}[os.environ.get('KWAR', '0')]

L = 2048
D = 1024
NT = 16
KC = 8
NH = 8
PROJ = 8208
NE = 16
FF = 256
ALPHA = 2.0 ** 0.25
LN_EPS = 1e-5
NORM_EPS = 1e-6
C_QKV, C_Z, C_A, C_UV, C_GDN, C_GSGU = 0, 3072, 4096, 4112, 6160, 7184


class Buf:
    __slots__ = ("name", "writers", "readers", "excl")

    def __init__(self, name, excl=False):
        self.name = name
        self.excl = excl
        self.writers = {}
        self.readers = {}


class Op:
    __slots__ = ("eng", "fn", "raw", "war", "is_dma", "signal", "count", "dsem", "dcount", "prev_dma", "key")

    def __init__(self, eng, fn, is_dma):
        self.eng = eng
        self.fn = fn
        self.raw = set()
        self.war = set()
        self.is_dma = is_dma
        self.signal = False
        self.count = None
        self.dsem = None
        self.dcount = None
        self.prev_dma = None
        self.key = eng


class Builder:
    def __init__(self, nc):
        self.nc = nc
        self.ops = []
        self.dma_rr = {e: 0 for e in ENGS}
        self.dma_last = {}

    def add(self, eng, fn, reads=(), writes=(), dma=False, accumulate=False):
        op = Op(eng, fn, dma)
        if dma:
            slot = self.dma_rr[eng] % NDMASEM
            self.dma_rr[eng] += 1
            op.dsem = (eng, slot)
            op.key = (eng, slot)
            prev = self.dma_last.get((eng, slot))
            op.prev_dma = prev
            op.dcount = (prev.dcount if prev is not None else 0) + 16
            self.dma_last[(eng, slot)] = op
        for b in reads:
            op.raw.update(b.writers.values())
            if b.excl:
                for kk, v in b.readers.items():
                    if v.eng != eng:
                        op.raw.add(v)
        for b in writes:
            if accumulate:
                op.war.update(v for v in b.writers.values() if not v.is_dma)
            else:
                op.war.update(b.writers.values())
            op.war.update(b.readers.values())
        for b in reads:
            b.readers[op.key] = op
        for b in writes:
            if accumulate:
                b.writers = {kk: v for kk, v in b.writers.items() if v.is_dma}
                b.writers[op.key] = op
            else:
                b.writers = {op.key: op}
            b.readers = {}
        self.ops.append(op)
        return op

    def emit(self):
        nc = self.nc
        ops = self.ops
        for op in ops:
            deps = set()
            for d in op.raw:
                if d is op:
                    continue
                if (not d.is_dma) and (not op.is_dma) and d.eng == op.eng and op.eng == PE:
                    continue
                deps.add(d)
            for d in op.war:
                if d is op or d in deps:
                    continue
                if (not d.is_dma) and (not op.is_dma) and d.eng == op.eng and (op.eng == PE or (op.eng in WAR_SKIP)):
                    continue
                deps.add(d)
            op.raw = deps
            for d in deps:
                if not d.is_dma:
                    d.signal = True
        self.maxwait = {}
        counts = {e: 0 for e in ENGS}
        for op in ops:
            if (not op.is_dma) and op.signal:
                counts[op.eng] += 1
                op.count = counts[op.eng]
        if os.environ.get('KVERBOSE'):
            print('signal counts', counts, 'nops', len(ops), 'dma', {k_: v.dcount for k_, v in self.dma_last.items()})
        with contextlib.ExitStack() as st:
            esem = {e: st.enter_context(nc.semaphore(f"s_{e}")) for e in (PE, DVE, ACT, POOL)}
            dsem = {}
            for e in (SP, POOL):
                for s in range(NDMASEM):
                    dsem[(e, s)] = st.enter_context(nc.semaphore(f"d_{e}_{s}"))
            block = st.enter_context(nc.Block())

            def run_engine(eng_name, eng):
                known = {}
                for op in ops:
                    if op.eng != eng_name:
                        continue
                    need = {}
                    for d in op.raw:
                        if d.is_dma:
                            key, val = ("d",) + d.dsem, d.dcount
                        else:
                            key, val = ("e", d.eng), d.count
                        if val > need.get(key, 0):
                            need[key] = val
                    if op.is_dma and op.prev_dma is not None:
                        key = ("d",) + op.dsem
                        need[key] = max(need.get(key, 0), op.prev_dma.dcount)
                    todo = []
                    for key, val in need.items():
                        if known.get(key, 0) >= val:
                            continue
                        known[key] = val
                        sem = dsem[key[1:]] if key[0] == "d" else esem[key[1]]
                        self.maxwait[key] = max(self.maxwait.get(key, 0), val)
                        todo.append((sem, val))
                    attach = None
                    if ATTACH and todo and not op.is_dma:
                        attach = todo.pop()
                    for sem, val in todo:
                        eng.wait_ge(sem, val)
                    ins = op.fn(eng)
                    if attach is not None:
                        ins._wait_ge(attach[0], attach[1])
                    if op.is_dma:
                        ins.then_inc(dsem[op.dsem], 16)
                    elif op.signal:
                        ins.then_inc(esem[op.eng], 1)
                if eng_name == SP:
                    for key, last in self.dma_last.items():
                        eng.wait_ge(dsem[key], last.dcount)

            @block.tensor
            def _(eng):
                run_engine(PE, eng)

            @block.vector
            def _(eng):
                run_engine(DVE, eng)

            @block.scalar
            def _(eng):
                run_engine(ACT, eng)

            @block.gpsimd
            def _(eng):
                run_engine(POOL, eng)

            @block.sync
            def _(eng):
                run_engine(SP, eng)


class Reg:
    def __init__(self, arena, off, n, buf):
        self.arena, self.off, self.n, self.buf = arena, off, n, buf

    def f(self, a=0, b=None):
        b = self.n if b is None else b
        return self.arena[:, self.off + a:self.off + b]

    def h(self, a=0, b=None):
        v = self.arena[:, self.off:self.off + self.n].bitcast(BF16)
        b = 2 * self.n if b is None else b
        return v[:, a:b]


class _GatedBuilder(Builder):
    def __init__(self, nc, kb):
        super().__init__(nc)
        self.kb = kb

    def add(self, *a, **kw):
        if not self.kb.enabled:
            return None
        return super().add(*a, **kw)


class KB:
    def __init__(self, nc, arena, arena_cols, psum):
        self.nc = nc
        self.B = _GatedBuilder(nc, self)
        self.arena = arena
        self.cols = arena_cols
        self.top = 0
        self.dead = []
        self.live = []
        self.registry = {}
        self.psum = psum
        self.pbufs = [Buf(f"ps{i}", excl=True) for i in range(8)]
        self.prr = 0
        self.enabled = True
        self.phase_on = True
        self.sub_on = True
        self.stop = float(os.environ.get('KSTOP', '99'))

    def alloc(self, name, n, at=None):
        n = (n + 7) // 8 * 8
        off = self.top if at is None else at
        assert off + n <= self.cols, f"arena overflow at {name}: {off + n} > {self.cols}"
        for r in self.live:
            assert not (r.off < off + n and off < r.off + r.n), f"{name} overlaps live {r.buf.name}"
        buf = Buf(name)
        for r in self.dead:
            if r.off < off + n and off < r.off + r.n:
                ob = r.buf
                for kk, v in ob.readers.items():
                    buf.readers[("x", id(ob), kk)] = v
                for kk, v in ob.writers.items():
                    buf.readers[("w", id(ob), kk)] = v
        reg = Reg(self.arena, off, n, buf)
        self.registry[name] = reg
        self.live.append(reg)
        self.top = off + n
        return reg

    def kill(self, regs):
        for r in regs:
            self.live.remove(r)
            self.dead.append(r)

    def sub(self, n):
        lim = float(os.environ.get('KSUB', '99'))
        if getattr(self, 'cur_hh', 0) == 1 and os.environ.get('KSUB1'):
            lim = float(os.environ['KSUB1'])
        self.sub_on = n <= lim
        self.enabled = self.sub_on and self.phase_on

    def phase(self, n):
        self.phase_on = n <= self.stop
        self.sub_on = True
        self.enabled = self.phase_on

    def bank(self, i=None):
        if i is None:
            i = self.prr % 8
            self.prr += 1
        return self.psum[:, i * 512:(i + 1) * 512], self.pbufs[i]

    def mm(self, out, lhsT, rhs, start, stop, r, w):
        self.B.add(PE, lambda e: e.matmul(out, lhsT=lhsT, rhs=rhs, start=start, stop=stop), r, w)

    def tr(self, out, in_, ident, r, w):
        self.B.add(PE, lambda e: e.matmul(out, lhsT=in_, rhs=ident, start=True, stop=True), r, w)

    def act(self, out, in_, func, r, w, bias=None, scale=None, accum=None):
        kw = {}
        if bias is not None:
            kw["bias"] = bias
        if scale is not None:
            kw["scale"] = scale
        if accum is not None:
            kw["accum_out"] = accum
        self.B.add(ACT, lambda e: e.activation(out=out, in_=in_, func=func, **kw), r, w)

    def tt(self, out, a, b, op, r, w, eng=DVE):
        self.B.add(eng, lambda e: e.tensor_tensor(out=out, in0=a, in1=b, op=op), r, w)

    def ts(self, out, a, s1, s2, op0, op1, r, w, eng=DVE):
        if op1 is None:
            self.B.add(eng, lambda e: e.tensor_scalar(out=out, in0=a, scalar1=s1, scalar2=None, op0=op0), r, w)
        else:
            self.B.add(eng, lambda e: e.tensor_scalar(out=out, in0=a, scalar1=s1, scalar2=s2, op0=op0, op1=op1), r, w)

    def stt(self, out, a, s, b, op0, op1, r, w, eng=DVE):
        self.B.add(eng, lambda e: e.scalar_tensor_tensor(out=out, in0=a, scalar=s, in1=b, op0=op0, op1=op1), r, w)

    def rsqrt(self, out, in_, eps, r, w):
        self.ts(out, in_, eps, None, ALU.add, None, r, w)
        self.act(out, out, AF.Ln, w, w)
        self.act(out, out, AF.Exp, w, w, scale=-0.5)

    def cp(self, out, in_, r, w, eng=DVE):
        self.B.add(eng, lambda e: e.tensor_copy(out=out, in_=in_), r, w)

    def memset(self, out, val, w, eng=POOL):
        self.B.add(eng, lambda e: e.memset(out, val), (), w)

    def dma(self, out, in_, r, w, q=SP, acc=False):
        self.B.add(q, lambda e: e.dma_start(out=out, in_=in_), r, w, dma=True, accumulate=acc)

    def generic(self, eng, fn, r, w):
        self.B.add(eng, fn, r, w)


def _consts():
    i = np.arange(128)
    t = i[:, None]
    s = i[None, :]
    tabs = {}
    tabs["ident"] = (t == s)
    tabs["U"] = (t <= s)
    tabs["SL"] = (t > s)
    tabs["UI"] = (t <= s)
    tabs["ones"] = np.ones((128, 128), bool)
    tabs["D16"] = (t // 16 == s // 16) & (t > s)
    tabs["D16T"] = tabs["D16"].T
    for b in (16, 32, 64):
        em = (t // (2 * b) == s // (2 * b)) & ((t % (2 * b)) >= b) & ((s % (2 * b)) < b)
        tabs[f"E{b}"] = em
        tabs[f"F{b}"] = em.T
    names = list(tabs)
    arr = np.concatenate([tabs[n].astype(np.float32) for n in names], axis=1)
    return names, np.ascontiguousarray(arr)


CNAMES, CARR = _consts()


def build_program(debug=None):
    nc = bass.Bass("TRN2", target_bir_lowering=False)

    def din(name, shape):
        return nc.dram_tensor(name, list(shape), F32, kind="ExternalInput").ap()

    xT_d = din("xT", (D, L))
    x_d = din("x", (L, D))
    w_in = din("w_in", (D, PROJ))
    convT = din("convT", (128, 24 * 4))
    a_log = din("a_log", (1, NH))
    dt_bias = din("dt_bias", (1, NH))
    dn_norm_g = din("dn_norm_g", (1, 128))
    w_dn_out = din("w_dn_out", (D, D))
    sgu_ln_g = din("sgu_ln_g", (1, D))
    sgu_ln_b = din("sgu_ln_b", (1, D))
    spwT = din("spwT", (8, 128, 128))
    spb = din("spb", (1, 8 * 128))
    w_sgu_out = din("w_sgu_out", (D, D))
    w_out = din("w_out", (D, D))
    ln1_g = din("ln1_g", (1, D))
    ln1_b = din("ln1_b", (1, D))
    w_rt = din("w_rt", (D, 20))
    b_rt = din("b_rt", (1, 20))
    ew_gate = din("ew_gate", (NE, D, FF))
    ew_up = din("ew_up", (NE, D, FF))
    ew_down = din("ew_down", (NE, FF, D))
    ln2_g = din("ln2_g", (1, D))
    ln2_b = din("ln2_b", (1, D))
    ctab = din("ctab", (128, CARR.shape[1]))
    out_d = nc.dram_tensor("out", [L, D], F32, kind="ExternalOutput").ap()
    h1_d = nc.dram_tensor("h1s", [L, D], F32, kind="Internal").ap()
    dbg = {}
    if debug:
        for name, shape in debug.items():
            dbg[name] = nc.dram_tensor("dbg_" + name, list(shape), F32, kind="ExternalOutput").ap()

    AW = 53200
    with contextlib.ExitStack() as st:
        arena = st.enter_context(nc.sbuf_tensor("arena", [128, AW], F32))
        psum = st.enter_context(nc.psum_tensor("psum", [128, 4096], F32))
        k = KB(nc, arena, AW, psum)
        h1bufs = [Buf(f"h1d{i}") for i in range(NT)]

        def bcast_load(reg, src_row, n, a=0):
            k.dma(reg.f(a, a + n).unsqueeze(1), src_row.partition_broadcast(128), (), [reg.buf], acc=True)

        if os.environ.get('KZERO', '0') == '1':
            zb = Buf('zero')
            for z0 in range(0, AW, 6400):
                k.memset(arena[:, z0:z0 + 6400], 0.0, [zb], eng=DVE if (z0 // 6400) % 2 else POOL)
            k.dead.append(Reg(arena, 0, AW, zb))
        nct = len(CNAMES)
        cf = k.alloc("cf", nct * 128)
        k.dma(cf.f(), ctab, (), [cf.buf])
        cb = k.alloc("cb", nct * 64)
        k.dma(cb.h(), ctab, (), [cb.buf], q=POOL)

        def CF(name):
            j = CNAMES.index(name)
            return cf.f(j * 128, (j + 1) * 128)

        def CBh(name):
            j = CNAMES.index(name)
            return cb.h(j * 128, (j + 1) * 128)

        xT = k.alloc("xT", KC * L // 2)
        xTv = xT.h().rearrange("p (c t) -> p c t", t=L)
        for c in range(KC):
            k.dma(xTv[:, c, :], xT_d[c * 128:(c + 1) * 128, :], (), [xT.buf], q=POOL, acc=True)
        oT = k.alloc("oT", NH * L // 2)
        oTv = oT.h().rearrange("p (h t) -> p h t", t=L)

        mark_dn = k.top
        k.phase(1)
        small = k.alloc("small", 11 * 128)
        sm = lambda j: small.f(j * 128, (j + 1) * 128)
        G_, BETA, EG, EGL, EGLMG, BEG, GG, TMP, TMP2, AB0, AB1 = range(11)
        prm = k.alloc("prm", 16 + 128)
        bcast_load(prm, a_log, NH, 0)
        bcast_load(prm, dt_bias, NH, 8)
        bcast_load(prm, dn_norm_g, 128, 16)
        wab = k.alloc("wab", KC * 16 // 2)
        wabv = wab.h().rearrange("p (c n) -> p c n", n=16)
        k.dma(wabv, w_in[:, C_A:C_A + 16].rearrange("(c p) n -> p c n", p=128), (), [wab.buf], q=POOL)
        abv = small.f(AB0 * 128, AB0 * 128 + 256).rearrange("p (t n) -> p t n", n=16)
        for i in range(NT):
            pb, pbuf = k.bank()
            for c in range(KC):
                k.mm(pb[:, 0:16], xTv[:, c, i * 128:(i + 1) * 128], wabv[:, c, :], c == 0, c == KC - 1,
                     [xT.buf, wab.buf], [pbuf])
            k.act(abv[:, i, :], pb[:, 0:16], AF.Copy, [pbuf], [small.buf])
        a_v = abv[:, :, 0:8]
        b_v = abv[:, :, 8:16]
        v3 = lambda j: sm(j).rearrange("p (t n) -> p t n", n=8)
        sb = [small.buf]
        k.tt(v3(TMP), a_v, prm.f(8, 16).unsqueeze(1).to_broadcast([128, NT, 8]), ALU.add, sb + [prm.buf], sb)
        k.act(sm(TMP), sm(TMP), AF.Exp, sb, sb)
        k.ts(sm(TMP), sm(TMP), 1.0, None, ALU.add, None, sb, sb)
        k.act(sm(TMP), sm(TMP), AF.Ln, sb, sb)
        k.act(prm.f(0, 8), prm.f(0, 8), AF.Exp, [prm.buf], [prm.buf])
        k.stt(v3(GG), v3(TMP), -1.0, prm.f(0, 8).unsqueeze(1).to_broadcast([128, NT, 8]), ALU.mult, ALU.mult,
              sb + [prm.buf], sb)
        k.act(v3(BETA), b_v, AF.Exp, sb, sb, scale=-1.0)
        k.ts(sm(BETA), sm(BETA), 1.0, None, ALU.add, None, sb, sb)
        k.generic(DVE, lambda e: e.reciprocal(out=sm(BETA), in_=sm(BETA)), sb, sb)
        pb, pbuf = k.bank()
        k.mm(pb[:, 0:128], CF("U"), sm(GG), True, True, [cf.buf] + sb, [pbuf])
        k.mm(pb[:, 128:256], CF("ones"), sm(GG), True, True, [cf.buf] + sb, [pbuf])
        k.act(sm(G_), pb[:, 0:128], AF.Copy, [pbuf], sb)
        k.act(sm(EG), pb[:, 0:128], AF.Exp, [pbuf], sb)
        k.act(sm(EGL), pb[:, 128:256], AF.Exp, [pbuf], sb)
        k.tt(sm(TMP2), pb[:, 128:256], sm(G_), ALU.subtract, [pbuf] + sb, sb)
        k.act(sm(EGLMG), sm(TMP2), AF.Exp, sb, sb)
        k.tt(sm(BEG), sm(BETA), sm(EG), ALU.mult, sb, sb)
        k.ts(prm.f(16, 144), prm.f(16, 144), float(np.sqrt(128.0)), None, ALU.mult, None, [prm.buf], [prm.buf])

        if os.environ.get('KNAN'):
            nn = k.alloc('nantest', 8)
            k.memset(nn.f(), -1.0, [nn.buf])
            k.act(nn.f(), nn.f(), AF.Ln, [nn.buf], [nn.buf])
            k.kill([nn])
            k.top = nn.off
        k.phase(2)
        cw = k.alloc("cw", 96)
        k.dma(cw.f(), convT, (), [cw.buf])
        cinL = [k.alloc(f"cin{j}", (4 + L + 12) // 2) for j in range(2)]
        dgL = [k.alloc(f"dg{j}", 4 * 64) for j in range(2)]
        caccL = [k.alloc(f"cacc{j}", L) for j in range(2)]
        csqL = [k.alloc(f"csq{j}", L // 2) for j in range(2)]
        _crn = k.alloc("crn", 512)
        crnL = [_crn, _crn]
        for cin_ in cinL:
            k.memset(cin_.h(0, 4), 0.0, [cin_.buf])
        _wp = k.alloc("wp", 4 * KC * 256 // 2)
        wp = [_wp, _wp]
        qkv = k.alloc("qkvT", 6 * L // 2)
        qkvv = qkv.h().rearrange("p (c t) -> p c t", t=L)
        szr = k.alloc("sz", NT * 256 // 2)
        szv = szr.h().rearrange("p (t n) -> p t n", n=256)
        S32 = k.alloc("S32", 2 * 128)
        Sbf = k.alloc("Sbf", 2 * 64)
        NU = 4
        UNIT_REGS = (("tA", 64), ("tB", 64), ("tA1", 64), ("tB1", 64), ("tA2", 128), ("tE0", 64), ("tE1", 64),
                     ("tE2", 64), ("tF0", 64), ("tF1", 64), ("tR", 64), ("tT", 64), ("tZ", 128), ("tD", 256),
                     ("tX", 256), ("tGUh", 128), ("tqk", 64), ("tkbd", 64), ("tkdec", 64),
                     ("tvb", 64), ("tu", 128), ("tWT", 64))
        UT = [dict() for _ in range(NU)]
        for nm, n in UNIT_REGS:
            for u in range(NU):
                UT[u][nm] = k.alloc(f"{nm}_{u}", n)

        def gview(nm):
            r0 = UT[0][nm]
            return arena[:, r0.off:r0.off + NU * r0.n].bitcast(BF16).rearrange("p (u c) -> p u c", c=128)

        def gbufs(nm):
            return [UT[u][nm].buf for u in range(NU)]
        PT = [{nm: k.alloc(f"{nm}_p{u}", n) for nm, n in (("tvn", 64), ("to1", 128), ("to", 128), ("tof", 64), ("tss", 8))}
              for u in range(2)]

        def load_pair_weights(p, wr):
            wv = wr.h().rearrange("p (j c n) -> p j c n", j=4, n=256)
            for j, c0 in enumerate((C_QKV + p * 256, C_QKV + 1024 + p * 256, C_QKV + 2048 + p * 256, C_Z + p * 256)):
                k.dma(wv[:, j], w_in[:, c0:c0 + 256].rearrange("(c p) n -> p c n", p=128), (), [wr.buf], q=POOL, acc=True)
            return wv

        for p in range(int(os.environ.get('KPAIRS', '4'))):
            k.phase(2.1)
            wr = wp[0]
            wv = wv_next if p > 0 else load_pair_weights(0, wr)
            identb_ = CBh("ident")
            for ct in range(6):
                j, hh = ct // 2, ct % 2
                gct = j * 8 + 2 * p + hh
                cin, cacc, csq = cinL[ct % 2], caccL[ct % 2], csqL[ct % 2]
                dg = dgL[ct % 2]
                for tap in range(4):
                    k.ts(dg.h(tap * 128, (tap + 1) * 128), identb_, cw.f(gct * 4 + tap, gct * 4 + tap + 1), None, ALU.mult, None,
                         [cb.buf, cw.buf], [dg.buf])
                for tb in range(4):
                    pb, pbuf = k.bank()
                    for c in range(KC):
                        k.mm(pb, wv[:, j, c, hh * 128:(hh + 1) * 128], xTv[:, c, tb * 512:(tb + 1) * 512],
                             c == 0, c == KC - 1, [wr.buf, xT.buf], [pbuf])
                    k.act(cin.h(4 + tb * 512, 4 + (tb + 1) * 512), pb, AF.Copy, [pbuf], [cin.buf])
                for tb in range(4):
                    pb, pbuf = k.bank()
                    for tap in range(4):
                        k.mm(pb, dg.h(tap * 128, (tap + 1) * 128), cin.h(1 + tb * 512 + tap, 1 + tb * 512 + tap + 512),
                             tap == 0, tap == 3, [dg.buf, cin.buf], [pbuf])
                    if j == 2:
                        k.act(qkvv[:, ct, tb * 512:(tb + 1) * 512], pb, AF.Silu, [pbuf], [qkv.buf])
                    else:
                        k.act(cacc.f(tb * 512, (tb + 1) * 512), pb, AF.Silu, [pbuf], [cacc.buf])
                if j != 2:
                    k.act(csq.h(), cacc.f(), AF.Square, [cacc.buf], [csq.buf])
                    for tb in range(4):
                        pb, pbuf = k.bank()
                        crn = crnL[tb % 2]
                        k.mm(pb, CBh("ones"), csq.h(tb * 512, (tb + 1) * 512), True, True, [cb.buf, csq.buf], [pbuf])
                        k.rsqrt(crn.f(), pb, NORM_EPS, [pbuf], [crn.buf])
                        k.stt(qkvv[:, ct, tb * 512:(tb + 1) * 512], cacc.f(tb * 512, (tb + 1) * 512),
                              (128.0 ** -0.5) if j == 0 else 1.0, crn.f(), ALU.mult, ALU.mult,
                              [cacc.buf, crn.buf], [qkv.buf])
            k.phase(2.2)
            for i in range(NT):
                pb, pbuf = k.bank()
                for c in range(KC):
                    k.mm(pb[:, 0:256], xTv[:, c, i * 128:(i + 1) * 128], wv[:, 3, c, :], c == 0, c == KC - 1,
                         [xT.buf, wr.buf], [pbuf])
                k.act(szv[:, i, :], pb[:, 0:256], AF.Silu, [pbuf], [szr.buf])
            if p < 3:
                wv_next = load_pair_weights(p + 1, wr)
            k.memset(S32.f(), 0.0, [S32.buf])
            k.memset(Sbf.h(), 0.0, [Sbf.buf])
            identb = CBh("ident")

            def pre(i, hh, T):
                ts_ = slice(i * 128, (i + 1) * 128)
                h = 2 * p + hh
                col = i * 8 + h
                sc = lambda j: sm(j)[:, col:col + 1]
                qT = qkvv[:, 0 + hh, ts_]
                kT = qkvv[:, 2 + hh, ts_]
                vT = qkvv[:, 4 + hh, ts_]
                tA, tB, tA1, tB1, tA2, tR, tT, tZ, tD, tX = (T[n] for n in ("tA", "tB", "tA1", "tB1", "tA2", "tR", "tT", "tZ", "tD", "tX"))
                tGUh, tqk, tkbd, tkdec, tvb, tu, tWT = (T[n] for n in ("tGUh", "tqk", "tkbd", "tkdec", "tvb", "tu", "tWT"))
                tE = [T["tE0"], T["tE1"], T["tE2"]]
                tF = [T["tF0"], T["tF1"]]
                pb, pbuf = k.bank()
                k.tr(pb[:, 0:128], kT, identb, [qkv.buf, cb.buf], [pbuf])
                k.tr(pb[:, 128:256], vT, identb, [qkv.buf, cb.buf], [pbuf])
                k.act(tkbd.h(), pb[:, 0:128], AF.Identity, [pbuf, small.buf], [tkbd.buf], scale=sc(BEG))
                k.act(tvb.h(), pb[:, 128:256], AF.Identity, [pbuf, small.buf], [tvb.buf], scale=sc(BETA))
                k.ts(tkdec.h(), pb[:, 0:128], sc(EGLMG), None, ALU.mult, None, [pbuf, small.buf], [tkdec.buf])
                k.act(tGUh.h(0, 128), CF("U"), AF.Identity, [cf.buf, small.buf], [tGUh.buf], scale=sc(GG))
                k.stt(tGUh.h(128, 256), CF("U"), sc(GG), tGUh.h(0, 128), ALU.mult, ALU.subtract,
                      [cf.buf, small.buf, tGUh.buf], [tGUh.buf])
                yield
                pb, pbuf = k.bank()
                k.mm(pb[:, 0:128], kT, kT, True, True, [qkv.buf], [pbuf])
                k.mm(pb[:, 128:256], kT, qT, True, True, [qkv.buf], [pbuf])
                k.mm(pb[:, 256:384], tGUh.h(0, 128), CBh("SL"), True, False, [tGUh.buf, cb.buf], [pbuf])
                k.mm(pb[:, 256:384], tGUh.h(128, 256), CBh("SL"), False, True, [tGUh.buf, cb.buf], [pbuf])
                k.mm(pb[:, 384:512], CBh("SL"), tGUh.h(0, 128), True, False, [tGUh.buf, cb.buf], [pbuf])
                k.mm(pb[:, 384:512], CBh("SL"), tGUh.h(128, 256), False, True, [tGUh.buf, cb.buf], [pbuf])
                k.act(tD.f(), pb[:, 256:512], AF.Exp, [pbuf], [tD.buf])
                k.tt(tX.f(), tD.f(), pb[:, 0:256], ALU.mult, [tD.buf, pbuf], [tX.buf])
                k.stt(tA.h(), tX.f(0, 128), sc(BETA), CF("SL"), ALU.mult, ALU.mult,
                      [tX.buf, small.buf, cf.buf], [tA.buf])
                k.tt(tqk.h(), tX.f(128, 256), CF("UI"), ALU.mult, [tX.buf, cf.buf], [tqk.buf], eng=POOL)
                yield
                pb, pbuf = k.bank()
                k.tr(pb[:, 0:128], tA.h(), identb, [tA.buf, cb.buf], [pbuf])
                k.act(tB.h(), pb[:, 0:128], AF.Copy, [pbuf], [tB.buf])
                yield

            def group_masks():
                def bc(name):
                    return CBh(name).unsqueeze(1).to_broadcast([128, NU, 128])
                k.tt(gview("tA1"), gview("tA"), bc("D16"), ALU.mult, gbufs("tA") + [cb.buf], gbufs("tA1"))
                k.tt(gview("tB1"), gview("tB"), bc("D16T"), ALU.mult, gbufs("tB") + [cb.buf], gbufs("tB1"))
                k.tt(gview("tR"), bc("ident"), gview("tB1"), ALU.subtract, gbufs("tB1") + [cb.buf], gbufs("tR"))
                k.tt(gview("tT"), bc("ident"), gview("tA1"), ALU.subtract, gbufs("tA1") + [cb.buf], gbufs("tT"))
                for li, b in enumerate((16, 32, 64)):
                    k.tt(gview(f"tE{li}"), gview("tA"), bc(f"E{b}"), ALU.mult, gbufs("tA") + [cb.buf], gbufs(f"tE{li}"))
                    if b < 64:
                        k.tt(gview(f"tF{li}"), gview("tB"), bc(f"F{b}"), ALU.mult, gbufs("tB") + [cb.buf], gbufs(f"tF{li}"))

            def pre_b(i, hh, T):
                tA, tB, tA1, tB1, tA2, tR, tT, tZ = (T[n] for n in ("tA", "tB", "tA1", "tB1", "tA2", "tR", "tT", "tZ"))
                tkbd, tvb, tu, tWT = (T[n] for n in ("tkbd", "tvb", "tu", "tWT"))
                tE = [T["tE0"], T["tE1"], T["tE2"]]
                tF = [T["tF0"], T["tF1"]]
                curA, curB, curAb, curBb = tA1.h(), tB1.h(), tA1.buf, tB1.buf
                for lev in range(3):
                    pb, pbuf = k.bank()
                    k.mm(pb[:, 0:128], curB, curA, True, True, [curAb, curBb], [pbuf])
                    k.mm(pb[:, 128:256], curA, curB, True, True, [curAb, curBb], [pbuf])
                    k.act(tA2.h(), pb[:, 0:256], AF.Copy, [pbuf], [tA2.buf])
                    yield
                    nA, nB = tA2.h(0, 128), tA2.h(128, 256)
                    pb2, pbuf2 = k.bank()
                    k.mm(pb2[:, 0:128], nA, tR.h(), True, True, [tA2.buf, tR.buf], [pbuf2])
                    k.mm(pb2[:, 128:256], nB, tT.h(), True, True, [tA2.buf, tT.buf], [pbuf2])
                    k.tt(tR.h(), tR.h(), pb2[:, 0:128], ALU.add, [tR.buf, pbuf2], [tR.buf])
                    k.tt(tT.h(), tT.h(), pb2[:, 128:256], ALU.add, [tT.buf, pbuf2], [tT.buf])
                    curA, curB, curAb, curBb = tA2.h(0, 128), tA2.h(128, 256), tA2.buf, tA2.buf
                    yield
                for li, b in enumerate((16, 32, 64)):
                    pb, pbuf = k.bank()
                    k.mm(pb[:, 0:128], tE[li].h(), tR.h(), True, True, [tE[li].buf, tR.buf], [pbuf])
                    if b < 64:
                        k.mm(pb[:, 128:256], tF[li].h(), tT.h(), True, True, [tF[li].buf, tT.buf], [pbuf])
                        k.act(tZ.h(), pb[:, 0:256], AF.Copy, [pbuf], [tZ.buf])
                    else:
                        k.act(tZ.h(0, 128), pb[:, 0:128], AF.Copy, [pbuf], [tZ.buf])
                    yield
                    pb2, pbuf2 = k.bank()
                    k.mm(pb2[:, 0:128], tT.h(), tZ.h(0, 128), True, True, [tT.buf, tZ.buf], [pbuf2])
                    if b < 64:
                        k.mm(pb2[:, 128:256], tR.h(), tZ.h(128, 256), True, True, [tR.buf, tZ.buf], [pbuf2])
                    k.tt(tR.h(), tR.h(), pb2[:, 0:128], ALU.subtract, [tR.buf, pbuf2], [tR.buf])
                    if b < 64:
                        k.tt(tT.h(), tT.h(), pb2[:, 128:256], ALU.subtract, [tT.buf, pbuf2], [tT.buf])
                    yield
                pb, pbuf = k.bank()
                k.mm(pb[:, 0:128], tR.h(), tvb.h(), True, True, [tR.buf, tvb.buf], [pbuf])
                k.mm(pb[:, 128:256], tkbd.h(), tR.h(), True, True, [tR.buf, tkbd.buf], [pbuf])
                k.act(tu.f(), pb[:, 0:128], AF.Copy, [pbuf], [tu.buf])
                k.act(tWT.h(), pb[:, 128:256], AF.Copy, [pbuf], [tWT.buf])
                yield

            def post(i, hh, T, P):
                ts_ = slice(i * 128, (i + 1) * 128)
                h = 2 * p + hh
                col = i * 8 + h
                sc = lambda j: sm(j)[:, col:col + 1]
                qT = qkvv[:, 0 + hh, ts_]
                tqk, tkdec, tu, tWT = T["tqk"], T["tkdec"], T["tu"], T["tWT"]
                tvn, to1, to, tof, tss = P["tvn"], P["to1"], P["to"], P["tof"], P["tss"]
                Sh = Sbf.h(hh * 128, (hh + 1) * 128)
                Sf = S32.f(hh * 128, (hh + 1) * 128)
                pb, pbuf = k.bank()
                k.mm(pb[:, 0:128], tWT.h(), Sh, True, True, [tWT.buf, Sbf.buf], [pbuf])
                k.mm(pb[:, 128:256], qT, Sh, True, True, [qkv.buf, Sbf.buf], [pbuf])
                k.tt(tvn.h(), tu.f(), pb[:, 0:128], ALU.subtract, [tu.buf, pbuf], [tvn.buf])
                k.act(to1.f(), pb[:, 128:256], AF.Identity, [pbuf, small.buf], [to1.buf], scale=sc(EG))
                yield
                pb2, pbuf2 = k.bank()
                k.mm(pb2[:, 0:128], tqk.h(), tvn.h(), True, True, [tqk.buf, tvn.buf], [pbuf2])
                k.mm(pb2[:, 128:256], tkdec.h(), tvn.h(), True, True, [tkdec.buf, tvn.buf], [pbuf2])
                k.stt(Sf, Sf, sc(EGL), pb2[:, 128:256], ALU.mult, ALU.add, [S32.buf, small.buf, pbuf2], [S32.buf])
                k.act(Sh, Sf, AF.Copy, [S32.buf], [Sbf.buf])
                k.tt(to.f(), to1.f(), pb2[:, 0:128], ALU.add, [to1.buf, pbuf2], [to.buf])
                yield
                k.act(to1.f(), to.f(), AF.Square, [to.buf, tss.buf], [to1.buf, tss.buf], accum=tss.f(0, 1))
                k.rsqrt(tss.f(1, 2), tss.f(0, 1), 128.0 * NORM_EPS, [tss.buf], [tss.buf])
                k.stt(to.f(), to.f(), tss.f(1, 2), prm.f(16, 144), ALU.mult, ALU.mult,
                      [to.buf, tss.buf, prm.buf], [to.buf])
                k.tt(tof.h(), to.f(), szv[:, i, hh * 128:(hh + 1) * 128], ALU.mult, [to.buf, szr.buf], [tof.buf], eng=POOL)
                yield
                pb, pbuf = k.bank()
                k.tr(pb[:, 0:128], tof.h(), identb, [tof.buf, cb.buf], [pbuf])
                k.act(oTv[:, h, ts_], pb[:, 0:128], AF.Copy, [pbuf], [oT.buf])
                yield

            def lockstep(gens):
                gens = list(gens)
                while gens:
                    nxt = []
                    for g_ in gens:
                        try:
                            next(g_)
                            nxt.append(g_)
                        except StopIteration:
                            pass
                    gens = nxt

            TG = NU // 2
            for g0 in range(0, NT, TG):
                units = [(i, hh) for i in range(g0, g0 + TG) for hh in range(2)]
                lockstep(pre(i, hh, UT[u]) for u, (i, hh) in enumerate(units))
                group_masks()
                lockstep(pre_b(i, hh, UT[u]) for u, (i, hh) in enumerate(units))
                for i in range(g0, g0 + TG):
                    lockstep(post(i, hh, UT[(i - g0) * 2 + hh], PT[hh]) for hh in range(2))

        if debug and "oT" in dbg:
            dt_ = k.alloc("dbgt", NH * L)
            k.cp(dt_.f(), oT.h(), [oT.buf], [dt_.buf])
            k.dma(dbg["oT"], dt_.f(), [dt_.buf], [])

        dn_regs = [small, prm, wab, cw, wp[0], qkv, szr, S32, Sbf] + cinL + caccL + csqL + [crnL[0]] + dgL
        for T_ in UT + PT:
            dn_regs += list(T_.values())
        k.kill(dn_regs)
        k.top = mark_dn

        k.phase(3)
        def layer_norm(xin, xbuf, gam, bet, pbufs, outs, obufs, stats):
            st6 = stats.f(0, 12).rearrange("p (a b) -> p a b", b=6)
            k.generic(DVE, lambda e: e.bn_stats(out=st6[:, 0, :], in_=xin[:, 0:512]), [xbuf], [stats.buf])
            k.generic(DVE, lambda e: e.bn_stats(out=st6[:, 1, :], in_=xin[:, 512:1024]), [xbuf], [stats.buf])
            k.generic(DVE, lambda e: e.bn_aggr(out=stats.f(12, 14), in_=stats.f(0, 12)), [stats.buf], [stats.buf])
            k.rsqrt(stats.f(14, 15), stats.f(13, 14), LN_EPS, [stats.buf], [stats.buf])
            k.ts(xin, xin, stats.f(12, 13), stats.f(14, 15), ALU.subtract, ALU.mult, [xbuf, stats.buf], [xbuf])
            k.tt(xin, xin, gam, ALU.mult, [xbuf] + pbufs, [xbuf])
            for o, ob in zip(outs, obufs):
                k.tt(o, xin, bet, ALU.add, [xbuf] + pbufs, [ob])

        lnp = k.alloc("lnp", 2 * D + 1024)
        SK = os.environ.get('KSKIP', '')
        if 'b' not in SK:
            bcast_load(lnp, sgu_ln_g, D, 0)
            bcast_load(lnp, sgu_ln_b, D, D)
            bcast_load(lnp, spb, 1024, 2 * D)
        wsp = k.alloc("wsp", 8 * 128)
        wspv = wsp.f().rearrange("p (g t) -> p g t", t=128)
        if 'a' not in SK:
            k.dma(wspv, spwT.rearrange("g s t -> s g t"), (), [wsp.buf])
        wspb = k.alloc("wspb", 8 * 64)
        wspbv = wspb.h().rearrange("p (g t) -> p g t", t=128)
        if 'c' not in SK:
            k.tt(wspbv, wspv, CF("UI").unsqueeze(1).to_broadcast([128, 8, 128]), ALU.mult, [wsp.buf, cf.buf], [wspb.buf])
        gT = k.alloc("gT", KC * L // 2)
        gTv = gT.h().rearrange("p (c t) -> p c t", t=L)
        wblk = [k.alloc(f"wblk{j}", KC * 512 // 2) for j in range(2)]
        vtmp = k.alloc("vtmp", D)
        stt_ = k.alloc("stats", 16)
        mark_vn = k.top
        vn = k.alloc("vn", NT * D // 2)
        vnv = vn.h().rearrange("p (t n) -> p t n", n=D)
        k.phase(3.1)
        nblk = 0
        for half in range(2):
            wr = wblk[nblk % 2]
            nblk += 1
            wvv = wr.h().rearrange("p (c n) -> p c n", n=512)
            c0 = C_UV + D + half * 512
            k.dma(wvv, w_in[:, c0:c0 + 512].rearrange("(c p) n -> p c n", p=128), (), [wr.buf], q=POOL)
            for i in range(NT):
                pb, pbuf = k.bank()
                for c in range(KC):
                    k.mm(pb, xTv[:, c, i * 128:(i + 1) * 128], wvv[:, c, :], c == 0, c == KC - 1, [xT.buf, wr.buf], [pbuf])
                k.act(vnv[:, i, half * 512:(half + 1) * 512], pb, AF.Gelu, [pbuf], [vn.buf])
        k.phase(3.2)
        for i in range(NT):
            k.act(vtmp.f(), vnv[:, i, :], AF.Copy, [vn.buf], [vtmp.buf])
            layer_norm(vtmp.f(), vtmp.buf, lnp.f(0, D), lnp.f(D, 2 * D), [lnp.buf], [vnv[:, i, :]], [vn.buf], stt_)
        k.phase(3.3)
        for half in range(2):
            wr = wblk[nblk % 2]
            nblk += 1
            wvv = wr.h().rearrange("p (c n) -> p c n", n=512)
            c0 = C_UV + half * 512
            k.dma(wvv, w_in[:, c0:c0 + 512].rearrange("(c p) n -> p c n", p=128), (), [wr.buf], q=POOL)
            for cc in range(4):
                for tb in range(4):
                    pb, pbuf = k.bank()
                    for c in range(KC):
                        k.mm(pb, wvv[:, c, cc * 128:(cc + 1) * 128], xTv[:, c, tb * 512:(tb + 1) * 512],
                             c == 0, c == KC - 1, [xT.buf, wr.buf], [pbuf])
                    k.act(gTv[:, half * 4 + cc, tb * 512:(tb + 1) * 512], pb, AF.Gelu, [pbuf], [gT.buf])
        k.phase(3.4)
        for i in range(NT):
            for gh in range(2):
                pb, pbuf = k.bank()
                for gq in range(4):
                    g = gh * 4 + gq
                    k.mm(pb[:, gq * 128:(gq + 1) * 128], vnv[:, i, g * 128:(g + 1) * 128], wspbv[:, g, :], True, True,
                         [vn.buf, wspb.buf], [pbuf])
                tmpv = vtmp.f(0, 512).rearrange("p (g t) -> p g t", t=128)
                k.tt(tmpv, pb.rearrange("p (g t) -> p g t", t=128),
                     lnp.f(2 * D + gh * 512, 2 * D + (gh + 1) * 512).rearrange("p (g t) -> p g t", t=128), ALU.add,
                     [pbuf, lnp.buf], [vtmp.buf])
                k.tt(gTv[:, gh * 4:(gh + 1) * 4, i * 128:(i + 1) * 128], gTv[:, gh * 4:(gh + 1) * 4, i * 128:(i + 1) * 128],
                     tmpv, ALU.mult, [gT.buf, vtmp.buf], [gT.buf])
        k.kill([vn, wblk[0], wblk[1], vtmp, stt_, lnp, wsp, wspb])
        k.top = mark_vn
        k.phase(4)
        yT = k.alloc("yT", KC * L // 2)
        yTv = yT.h().rearrange("p (c t) -> p c t", t=L)
        wm = [k.alloc(f"wm{j}", 4 * KC * 128 // 2) for j in range(2)]
        sg = [k.alloc(f"sg{j}", 512) for j in range(2)]
        for dc in range(KC):
            wr = wm[dc % 2]
            wmv = wr.h().rearrange("p (j c n) -> p j c n", j=4, n=128)
            srcs = (w_dn_out[:, dc * 128:(dc + 1) * 128], w_sgu_out[:, dc * 128:(dc + 1) * 128],
                    w_in[:, C_GDN + dc * 128:C_GDN + (dc + 1) * 128], w_in[:, C_GSGU + dc * 128:C_GSGU + (dc + 1) * 128])
            for j, s_ in enumerate(srcs):
                k.dma(wmv[:, j], s_.rearrange("(c p) n -> p c n", p=128), (), [wr.buf], q=POOL, acc=True)
            for tb in range(4):
                tsl = slice(tb * 512, (tb + 1) * 512)
                banks = [k.bank() for _ in range(4)]
                rhs_src = (oTv, gTv, xTv, xTv)
                rbufs = (oT.buf, gT.buf, xT.buf, xT.buf)
                for j in range(4):
                    pb, pbuf = banks[j]
                    for c in range(KC):
                        k.mm(pb, wmv[:, j, c, :], rhs_src[j][:, c, tsl], c == 0, c == KC - 1, [wr.buf, rbufs[j]], [pbuf])
                k.act(sg[0].f(), banks[2][0], AF.Sigmoid, [banks[2][1]], [sg[0].buf])
                k.act(sg[1].f(), banks[3][0], AF.Sigmoid, [banks[3][1]], [sg[1].buf])
                k.tt(sg[0].f(), sg[0].f(), banks[0][0], ALU.mult, [sg[0].buf, banks[0][1]], [sg[0].buf])
                k.tt(sg[1].f(), sg[1].f(), banks[1][0], ALU.mult, [sg[1].buf, banks[1][1]], [sg[1].buf])
                k.tt(yTv[:, dc, tsl], sg[0].f(), sg[1].f(), ALU.add, [sg[0].buf, sg[1].buf], [yT.buf])

        k.phase(5)
        k.kill([xT, oT, gT, wm[0], wm[1], sg[0], sg[1]])
        k.top = cb.off + cb.n
        comb = k.alloc("comb", NT * NE)
        combv = comb.f().rearrange("p (t e) -> p t e", e=NE)
        h1T = k.alloc("h1T", KC * L // 2)
        h1Tv = h1T.h().rearrange("p (c t) -> p c t", t=L)
        mark5 = k.top
        ln1p = k.alloc("ln1p", 2 * D)
        bcast_load(ln1p, ln1_g, D, 0)
        bcast_load(ln1p, ln1_b, D, D)
        wo = k.alloc("wo", KC * D // 2)
        wov = wo.h().rearrange("p (c n) -> p c n", n=D)
        k.dma(wov, w_out.rearrange("(c p) n -> p c n", p=128), (), [wo.buf], q=POOL)
        wrt = k.alloc("wrt", KC * 20 + 24)
        wrtv = wrt.f(0, KC * 20).rearrange("p (c n) -> p c n", n=20)
        k.dma(wrtv, w_rt.rearrange("(c p) n -> p c n", p=128), (), [wrt.buf])
        bcast_load(wrt, b_rt, 20, KC * 20)
        xres = [k.alloc(f"xres{j}", D) for j in range(2)]
        h1t = [k.alloc(f"h1t{j}", D) for j in range(2)]
        h1Tf = k.alloc("h1Tf", KC * 128)
        h1Tfv = h1Tf.f().rearrange("p (c t) -> p c t", t=128)
        stat1 = k.alloc("stat1", 16)
        rt = k.alloc("rt", 256)
        identf = CF("ident")
        for i in range(NT):
            xr, hb = xres[i % 2], h1t[i % 2]
            k.dma(xr.f(), x_d[i * 128:(i + 1) * 128, :], (), [xr.buf])
            for half in range(2):
                pb, pbuf = k.bank()
                for c in range(KC):
                    k.mm(pb, yTv[:, c, i * 128:(i + 1) * 128], wov[:, c, half * 512:(half + 1) * 512], c == 0, c == KC - 1,
                         [yT.buf, wo.buf], [pbuf])
                k.stt(hb.f(half * 512, (half + 1) * 512), xr.f(half * 512, (half + 1) * 512), ALPHA, pb, ALU.mult, ALU.add,
                      [xr.buf, pbuf], [hb.buf])
            layer_norm(hb.f(), hb.buf, ln1p.f(0, D), ln1p.f(D, 2 * D), [ln1p.buf], [hb.f()], [hb.buf], stat1)
            k.dma(h1_d[i * 128:(i + 1) * 128, :], hb.f(), [hb.buf], [h1bufs[i]])
            for c4 in range(2):
                pb, pbuf = k.bank()
                for cc in range(4):
                    c = c4 * 4 + cc
                    k.tr(pb[:, cc * 128:(cc + 1) * 128], hb.f(c * 128, (c + 1) * 128), identf, [hb.buf, cf.buf], [pbuf])
                k.act(h1Tv[:, c4 * 4:(c4 + 1) * 4, i * 128:(i + 1) * 128], pb.rearrange("p (c t) -> p c t", t=128), AF.Copy,
                      [pbuf], [h1T.buf])
                k.cp(h1Tfv[:, c4 * 4:(c4 + 1) * 4, :], pb.rearrange("p (c t) -> p c t", t=128), [pbuf], [h1Tf.buf])
            pb, pbuf = k.bank()
            for c in range(KC):
                k.mm(pb[:, 0:20], h1Tfv[:, c, :], wrtv[:, c, :], c == 0, c == KC - 1, [h1Tf.buf, wrt.buf], [pbuf])
            R_ = lambda a, b: rt.f(a, b)
            rb = [rt.buf]
            k.tt(R_(0, 20), pb[:, 0:20], wrt.f(KC * 20, KC * 20 + 20), ALU.add, [pbuf, wrt.buf], rb)
            k.generic(DVE, lambda e: e.reduce_max(out=R_(20, 21), in_=R_(0, 4), axis=AX.X), rb, rb)
            k.ts(R_(24, 28), R_(0, 4), R_(20, 21), None, ALU.is_equal, None, rb, rb)
            k.ts(R_(28, 32), R_(0, 4), R_(20, 21), None, ALU.subtract, None, rb, rb)
            k.memset(R_(21, 22), 0.0, rb)
            k.act(R_(28, 32), R_(28, 32), AF.Exp, rb, rb, accum=R_(21, 22))
            k.generic(DVE, lambda e: e.reciprocal(out=R_(22, 23), in_=R_(21, 22)), rb, rb)
            el = R_(4, 20).rearrange("p (g j) -> p g j", j=4)
            k.tt(R_(32, 48).rearrange("p (g j) -> p g j", j=4), el, R_(24, 28).unsqueeze(2).to_broadcast([128, 4, 4]),
                 ALU.mult, rb, rb)
            k.generic(DVE, lambda e: e.reduce_sum(out=R_(48, 52), in_=R_(32, 48).rearrange("p (g j) -> p j g", j=4),
                                                  axis=AX.X), rb, rb)
            k.generic(DVE, lambda e: e.reduce_max(out=R_(52, 53), in_=R_(48, 52), axis=AX.X), rb, rb)
            k.ts(R_(56, 60), R_(48, 52), R_(52, 53), None, ALU.is_equal, None, rb, rb)
            k.stt(R_(60, 64), R_(56, 60), -1e30, R_(48, 52), ALU.mult, ALU.add, rb, rb)
            k.generic(DVE, lambda e: e.reduce_max(out=R_(53, 54), in_=R_(60, 64), axis=AX.X), rb, rb)
            k.ts(R_(64, 68), R_(60, 64), R_(53, 54), None, ALU.is_equal, None, rb, rb)
            k.tt(R_(54, 55), R_(53, 54), R_(52, 53), ALU.subtract, rb, rb)
            k.act(R_(54, 55), R_(54, 55), AF.Exp, rb, rb)
            k.ts(R_(54, 55), R_(54, 55), 1.0, None, ALU.add, None, rb, rb)
            k.generic(DVE, lambda e: e.reciprocal(out=R_(68, 69), in_=R_(54, 55)), rb, rb)
            k.ts(R_(69, 70), R_(68, 69), -1.0, 1.0, ALU.mult, ALU.add, rb, rb)
            k.tt(R_(68, 70), R_(68, 70), R_(22, 23).to_broadcast([128, 2]), ALU.mult, rb, rb)
            k.ts(R_(72, 76), R_(56, 60), R_(68, 69), None, ALU.mult, None, rb, rb)
            k.stt(R_(72, 76), R_(64, 68), R_(69, 70), R_(72, 76), ALU.mult, ALU.add, rb, rb)
            k.tt(combv[:, i, :].rearrange("p (g j) -> p g j", j=4), R_(24, 28).unsqueeze(2).to_broadcast([128, 4, 4]),
                 R_(72, 76).unsqueeze(1).to_broadcast([128, 4, 4]), ALU.mult, rb, [comb.buf])

        k.phase(6)
        k.kill([yT, ln1p, wo, wrt, xres[0], xres[1], h1t[0], h1t[1], h1Tf, stat1, rt])
        k.top = mark5
        yacc = k.alloc("yacc", NT * D)
        yaccv = yacc.f().rearrange("p (t n) -> p t n", n=D)
        ew = [k.alloc(f"ew{j}", (2 * KC * FF + 2 * D) // 2) for j in range(2)]
        hT = [k.alloc(f"hT{j}", 2 * 512 // 2) for j in range(2)]
        sgl = [k.alloc(f"sgl{j}", 512) for j in range(2)]

        ln2p = k.alloc("ln2p", 2 * D)
        bcast_load(ln2p, ln2_g, D, 0)
        bcast_load(ln2p, ln2_b, D, D)
        hres = [k.alloc(f"hres{j}", D) for j in range(2)]
        stat2 = k.alloc("stat2", 16)
        yb = [Buf(f"yacc{i}") for i in range(NT)]
        for b_ in yb:
            b_.readers = dict(yacc.buf.readers)

        def final_tile(i):
            hr = hres[i % 2]
            k.dma(hr.f(), h1_d[i * 128:(i + 1) * 128, :], [h1bufs[i]], [hr.buf])
            k.stt(hr.f(), hr.f(), ALPHA, yaccv[:, i, :], ALU.mult, ALU.add, [hr.buf, yb[i]], [hr.buf])
            layer_norm(hr.f(), hr.buf, ln2p.f(0, D), ln2p.f(D, 2 * D), [ln2p.buf], [hr.f()], [hr.buf], stat2)
            k.dma(out_d[i * 128:(i + 1) * 128, :], hr.f(), [hr.buf], [])

        def load_expert(e, wr):
            v = wr.h()
            g = v[:, 0:KC * FF].rearrange("p (c n) -> p c n", n=FF)
            u = v[:, KC * FF:2 * KC * FF].rearrange("p (c n) -> p c n", n=FF)
            d = v[:, 2 * KC * FF:2 * KC * FF + 2 * D].rearrange("p (c n) -> p c n", n=D)
            k.dma(g, ew_gate[e].rearrange("(c p) n -> p c n", p=128), (), [wr.buf], q=POOL, acc=True)
            k.dma(u, ew_up[e].rearrange("(c p) n -> p c n", p=128), (), [wr.buf], q=POOL, acc=True)
            k.dma(d, ew_down[e].rearrange("(c p) n -> p c n", p=128), (), [wr.buf], q=POOL, acc=True)
            return g, u, d

        nxt = load_expert(0, ew[0])
        for e in range(NE):
            g_, u_, d_ = nxt
            wr = ew[e % 2]
            if e + 1 < NE:
                nxt = load_expert(e + 1, ew[(e + 1) % 2])
            for tb in range(4):
                tsl = slice(tb * 512, (tb + 1) * 512)
                hr = hT[tb % 2]
                hv = hr.h().rearrange("p (c t) -> p c t", t=512)
                for fc in range(2):
                    pg, pgb = k.bank()
                    pu, pub = k.bank()
                    for c in range(KC):
                        k.mm(pg, g_[:, c, fc * 128:(fc + 1) * 128], h1Tv[:, c, tsl], c == 0, c == KC - 1, [wr.buf, h1T.buf], [pgb])
                    for c in range(KC):
                        k.mm(pu, u_[:, c, fc * 128:(fc + 1) * 128], h1Tv[:, c, tsl], c == 0, c == KC - 1, [wr.buf, h1T.buf], [pub])
                    sl = sgl[fc]
                    k.act(sl.f(), pg, AF.Silu, [pgb], [sl.buf])
                    k.tt(hv[:, fc, :], sl.f(), pu, ALU.mult, [sl.buf, pub], [hr.buf])
                for ti in range(4):
                    i = tb * 4 + ti
                    for half in range(2):
                        pb, pbuf = k.bank()
                        for fc in range(2):
                            k.mm(pb, hv[:, fc, ti * 128:(ti + 1) * 128], d_[:, fc, half * 512:(half + 1) * 512], fc == 0, fc == 1,
                                 [hr.buf, wr.buf], [pbuf])
                        dst = yaccv[:, i, half * 512:(half + 1) * 512]
                        if e == 0:
                            k.ts(dst, pb, combv[:, i, e:e + 1], None, ALU.mult, None, [pbuf, comb.buf], [yb[i]])
                        else:
                            k.stt(dst, pb, combv[:, i, e:e + 1], dst, ALU.mult, ALU.add, [pbuf, comb.buf, yb[i]], [yb[i]])
                    if e == NE - 1:
                        final_tile(i)

        k.enabled = True
        for nm in [x for x in os.environ.get('KDUMP', '').split(',') if x]:
            rg = k.registry[nm]
            dd = nc.dram_tensor('dump_' + nm, [128, rg.n], F32, kind='ExternalOutput').ap()
            k.dma(dd, rg.f(), [rg.buf], [])
        k.B.emit()
        if os.environ.get('KVERBOSE'):
            print('maxwait', {kk: v for kk, v in k.B.maxwait.items() if kk[0] == 'e'})
    return nc


_NC_CACHE = {}


def _prep_inputs(inputs, b):
    f = lambda a: np.ascontiguousarray(np.asarray(a, dtype=np.float32))
    x = np.asarray(inputs["x"], dtype=np.float32)
    m = {}
    m["x"] = f(x[b])
    m["xT"] = f(x[b].T)
    m["w_in"] = f(inputs["w_in"][0])
    cwv = np.asarray(inputs["conv_w"], dtype=np.float32)[0]
    m["convT"] = f(cwv.T.reshape(24, 128, 4).transpose(1, 0, 2).reshape(128, 96))
    m["a_log"] = f(inputs["a_log"]).reshape(1, NH)
    m["dt_bias"] = f(inputs["dt_bias"]).reshape(1, NH)
    m["dn_norm_g"] = f(inputs["dn_norm_g"]).reshape(1, 128)
    m["w_dn_out"] = f(inputs["w_dn_out"][0])
    m["sgu_ln_g"] = f(inputs["sgu_ln_g"]).reshape(1, D)
    m["sgu_ln_b"] = f(inputs["sgu_ln_b"]).reshape(1, D)
    m["spwT"] = f(np.asarray(inputs["spatial_w"], dtype=np.float32)[0].transpose(0, 2, 1))
    m["spb"] = f(inputs["spatial_b"]).reshape(1, 1024)
    m["w_sgu_out"] = f(inputs["w_sgu_out"][0])
    m["w_out"] = f(inputs["w_out"][0])
    m["ln1_g"] = f(inputs["ln1_g"]).reshape(1, D)
    m["ln1_b"] = f(inputs["ln1_b"]).reshape(1, D)
    m["w_rt"] = f(np.concatenate([np.asarray(inputs["router_group_w"], dtype=np.float32)[0],
                                  np.asarray(inputs["router_expert_w"], dtype=np.float32)[0]], axis=1))
    m["b_rt"] = f(np.concatenate([np.asarray(inputs["router_group_b"], dtype=np.float32)[0],
                                  np.asarray(inputs["router_expert_b"], dtype=np.float32)[0]], axis=0)).reshape(1, 20)
    m["ew_gate"] = f(inputs["expert_w_gate"][0])
    m["ew_up"] = f(inputs["expert_w_up"][0])
    m["ew_down"] = f(inputs["expert_w_down"][0])
    m["ln2_g"] = f(inputs["ln2_g"]).reshape(1, D)
    m["ln2_b"] = f(inputs["ln2_b"]).reshape(1, D)
    m["ctab"] = CARR
    return m


def kernel(**inputs):
    if "nc" not in _NC_CACHE:
        _NC_CACHE["nc"] = build_program()
    nc = _NC_CACHE["nc"]
    shared = None
    in_maps = []
    for b in range(8):
        m = _prep_inputs(inputs, b) if shared is None else dict(shared)
        if shared is None:
            shared = m
        else:
            x = np.asarray(inputs["x"], dtype=np.float32)
            m["x"] = np.ascontiguousarray(x[b])
            m["xT"] = np.ascontiguousarray(x[b].T)
        in_maps.append(m)
    res = run_bass_kernel_spmd(nc, in_maps, core_ids=list(range(8)))
    out = np.stack([np.asarray(r["out"], dtype=np.float32) for r in res.results], axis=0)
    return out
```

```python
import contextlib
import os
import numpy as np
import concourse.bass as bass
import concourse.mybir as mybir
from concourse.bass_utils import run_bass_kernel_spmd

F32 = mybir.dt.float32
BF16 = mybir.dt.bfloat16
AF = mybir.ActivationFunctionType
ALU = mybir.AluOpType
AX = mybir.AxisListType

PE, DVE, ACT, POOL, SP = "tensor", "vector", "scalar", "gpsimd", "sync"
ENGS = (PE, DVE, ACT, POOL, SP)
NDMASEM = 8
ATTACH = os.environ.get('KATTACH', '1') == '1'
WAR_SKIP = {'0': (), 'DA': (DVE, ACT), 'DAP': (DVE, ACT, POOL)}[os.environ.get('KWAR', '0')]

L = 2048
D = 1024
NT = 16
KC = 8
NH = 8
PROJ = 8208
NE = 16
FF = 256
ALPHA = 2.0 ** 0.25
LN_EPS = 1e-5
NORM_EPS = 1e-6
C_QKV, C_Z, C_A, C_UV, C_GDN, C_GSGU = 0, 3072, 4096, 4112, 6160, 7184


class Buf:
    __slots__ = ("name", "writers", "readers", "excl")

    def __init__(self, name, excl=False):
        self.name = name
        self.excl = excl
        self.writers = {}
        self.readers = {}


class Op:
    __slots__ = ("eng", "fn", "raw", "war", "is_dma", "signal", "count", "dsem", "dcount", "prev_dma", "key")

    def __init__(self, eng, fn, is_dma):
        self.eng = eng
        self.fn = fn
        self.raw = set()
        self.war = set()
        self.is_dma = is_dma
        self.signal = False
        self.count = None
        self.dsem = None
        self.dcount = None
        self.prev_dma = None
        self.key = eng


class Builder:
    def __init__(self, nc):
        self.nc = nc
        self.ops = []
        self.dma_rr = {e: 0 for e in ENGS}
        self.dma_last = {}

    def add(self, eng, fn, reads=(), writes=(), dma=False, accumulate=False):
        op = Op(eng, fn, dma)
        if dma:
            slot = self.dma_rr[eng] % NDMASEM
            self.dma_rr[eng] += 1
            op.dsem = (eng, slot)
            op.key = (eng, slot)
            prev = self.dma_last.get((eng, slot))
            op.prev_dma = prev
            op.dcount = (prev.dcount if prev is not None else 0) + 16
            self.dma_last[(eng, slot)] = op
        for b in reads:
            op.raw.update(b.writers.values())
            if b.excl:
                for kk, v in b.readers.items():
                    if v.eng != eng:
                        op.raw.add(v)
        for b in writes:
            if accumulate:
                op.war.update(v for v in b.writers.values() if not v.is_dma)
            else:
                op.war.update(b.writers.values())
            op.war.update(b.readers.values())
        for b in reads:
            b.readers[op.key] = op
        for b in writes:
            if accumulate:
                b.writers = {kk: v for kk, v in b.writers.items() if v.is_dma}
                b.writers[op.key] = op
            else:
                b.writers = {op.key: op}
            b.readers = {}
        self.ops.append(op)
        return op

    def emit(self):
        nc = self.nc
        ops = self.ops
        for op in ops:
            deps = set()
            for d in op.raw:
                if d is op:
                    continue
                if (not d.is_dma) and (not op.is_dma) and d.eng == op.eng and op.eng == PE:
                    continue
                deps.add(d)
            for d in op.war:
                if d is op or d in deps:
                    continue
                if (not d.is_dma) and (not op.is_dma) and d.eng == op.eng and (op.eng == PE or (op.eng in WAR_SKIP)):
                    continue
                deps.add(d)
            op.raw = deps
            for d in deps:
                if not d.is_dma:
                    d.signal = True
        self.maxwait = {}
        counts = {e: 0 for e in ENGS}
        for op in ops:
            if (not op.is_dma) and op.signal:
                counts[op.eng] += 1
                op.count = counts[op.eng]
        if os.environ.get('KVERBOSE'):
            print('signal counts', counts, 'nops', len(ops), 'dma', {k_: v.dcount for k_, v in self.dma_last.items()})
        with contextlib.ExitStack() as st:
            esem = {e: st.enter_context(nc.semaphore(f"s_{e}")) for e in (PE, DVE, ACT, POOL)}
            dsem = {}
            for e in (SP, POOL):
                for s in range(NDMASEM):
                    dsem[(e, s)] = st.enter_context(nc.semaphore(f"d_{e}_{s}"))
            block = st.enter_context(nc.Block())

            def run_engine(eng_name, eng):
                known = {}
                for op in ops:
                    if op.eng != eng_name:
                        continue
                    need = {}
                    for d in op.raw:
                        if d.is_dma:
                            key, val = ("d",) + d.dsem, d.dcount
                        else:
                            key, val = ("e", d.eng), d.count
                        if val > need.get(key, 0):
                            need[key] = val
                    if op.is_dma and op.prev_dma is not None:
                        key = ("d",) + op.dsem
                        need[key] = max(need.get(key, 0), op.prev_dma.dcount)
                    todo = []
                    for key, val in need.items():
                        if known.get(key, 0) >= val:
                            continue
                        known[key] = val
                        sem = dsem[key[1:]] if key[0] == "d" else esem[key[1]]
                        self.maxwait[key] = max(self.maxwait.get(key, 0), val)
                        todo.append((sem, val))
                    attach = None
                    if ATTACH and todo and not op.is_dma:
                        attach = todo.pop()
                    for sem, val in todo:
                        eng.wait_ge(sem, val)
                    ins = op.fn(eng)
                    if attach is not None:
                        ins._wait_ge(attach[0], attach[1])
                    if op.is_dma:
                        ins.then_inc(dsem[op.dsem], 16)
                    elif op.signal:
                        ins.then_inc(esem[op.eng], 1)
                if eng_name == SP:
                    for key, last in self.dma_last.items():
                        eng.wait_ge(dsem[key], last.dcount)

            @block.tensor
            def _(eng):
                run_engine(PE, eng)

            @block.vector
            def _(eng):
                run_engine(DVE, eng)

            @block.scalar
            def _(eng):
                run_engine(ACT, eng)

            @block.gpsimd
            def _(eng):
                run_engine(POOL, eng)

            @block.sync
            def _(eng):
                run_engine(SP, eng)


class Reg:
    def __init__(self, arena, off, n, buf):
        self.arena, self.off, self.n, self.buf = arena, off, n, buf

    def f(self, a=0, b=None):
        b = self.n if b is None else b
        return self.arena[:, self.off + a:self.off + b]

    def h(self, a=0, b=None):
        v = self.arena[:, self.off:self.off + self.n].bitcast(BF16)
        b = 2 * self.n if b is None else b
        return v[:, a:b]


class _GatedBuilder(Builder):
    def __init__(self, nc, kb):
        super().__init__(nc)
        self.kb = kb

    def add(self, *a, **kw):
        if not self.kb.enabled:
            return None
        return super().add(*a, **kw)


class KB:
    def __init__(self, nc, arena, arena_cols, psum):
        self.nc = nc
        self.B = _GatedBuilder(nc, self)
        self.arena = arena
        self.cols = arena_cols
        self.top = 0
        self.dead = []
        self.live = []
        self.registry = {}
        self.psum = psum
        self.pbufs = [Buf(f"ps{i}", excl=True) for i in range(8)]
        self.prr = 0
        self.enabled = True
        self.phase_on = True
        self.sub_on = True
        self.stop = float(os.environ.get('KSTOP', '99'))

    def alloc(self, name, n, at=None):
        n = (n + 7) // 8 * 8
        off = self.top if at is None else at
        assert off + n <= self.cols, f"arena overflow at {name}: {off + n} > {self.cols}"
        for r in self.live:
            assert not (r.off < off + n and off < r.off + r.n), f"{name} overlaps live {r.buf.name}"
        buf = Buf(name)
        for r in self.dead:
            if r.off < off + n and off < r.off + r.n:
                ob = r.buf
                for kk, v in ob.readers.items():
                    buf.readers[("x", id(ob), kk)] = v
                for kk, v in ob.writers.items():
                    buf.readers[("w", id(ob), kk)] = v
        reg = Reg(self.arena, off, n, buf)
        self.registry[name] = reg
        self.live.append(reg)
        self.top = off + n
        return reg

    def kill(self, regs):
        for r in regs:
            self.live.remove(r)
            self.dead.append(r)

    def sub(self, n):
        lim = float(os.environ.get('KSUB', '99'))
        if getattr(self, 'cur_hh', 0) == 1 and os.environ.get('KSUB1'):
            lim = float(os.environ['KSUB1'])
        self.sub_on = n <= lim
        self.enabled = self.sub_on and self.phase_on

    def phase(self, n):
        self.phase_on = n <= self.stop
        self.sub_on = True
        self.enabled = self.phase_on

    def bank(self, i=None):
        if i is None:
            i = self.prr % 8
            self.prr += 1
        return self.psum[:, i * 512:(i + 1) * 512], self.pbufs[i]

    def mm(self, out, lhsT, rhs, start, stop, r, w):
        self.B.add(PE, lambda e: e.matmul(out, lhsT=lhsT, rhs=rhs, start=start, stop=stop), r, w)

    def tr(self, out, in_, ident, r, w):
        self.B.add(PE, lambda e: e.matmul(out, lhsT=in_, rhs=ident, start=True, stop=True), r, w)

    def act(self, out, in_, func, r, w, bias=None, scale=None, accum=None):
        kw = {}
        if bias is not None:
            kw["bias"] = bias
        if scale is not None:
            kw["scale"] = scale
        if accum is not None:
            kw["accum_out"] = accum
        self.B.add(ACT, lambda e: e.activation(out=out, in_=in_, func=func, **kw), r, w)

    def tt(self, out, a, b, op, r, w, eng=DVE):
        self.B.add(eng, lambda e: e.tensor_tensor(out=out, in0=a, in1=b, op=op), r, w)

    def ts(self, out, a, s1, s2, op0, op1, r, w, eng=DVE):
        if op1 is None:
            self.B.add(eng, lambda e: e.tensor_scalar(out=out, in0=a, scalar1=s1, scalar2=None, op0=op0), r, w)
        else:
            self.B.add(eng, lambda e: e.tensor_scalar(out=out, in0=a, scalar1=s1, scalar2=s2, op0=op0, op1=op1), r, w)

    def stt(self, out, a, s, b, op0, op1, r, w, eng=DVE):
        self.B.add(eng, lambda e: e.scalar_tensor_tensor(out=out, in0=a, scalar=s, in1=b, op0=op0, op1=op1), r, w)

    def rsqrt(self, out, in_, eps, r, w):
        self.ts(out, in_, eps, None, ALU.add, None, r, w)
        self.act(out, out, AF.Ln, w, w)
        self.act(out, out, AF.Exp, w, w, scale=-0.5)

    def cp(self, out, in_, r, w, eng=DVE):
        self.B.add(eng, lambda e: e.tensor_copy(out=out, in_=in_), r, w)

    def memset(self, out, val, w, eng=POOL):
        self.B.add(eng, lambda e: e.memset(out, val), (), w)

    def dma(self, out, in_, r, w, q=SP, acc=False):
        self.B.add(q, lambda e: e.dma_start(out=out, in_=in_), r, w, dma=True, accumulate=acc)

    def generic(self, eng, fn, r, w):
        self.B.add(eng, fn, r, w)


def _consts():
    i = np.arange(128)
    t = i[:, None]
    s = i[None, :]
    tabs = {}
    tabs["ident"] = (t == s)
    tabs["U"] = (t <= s)
    tabs["SL"] = (t > s)
    tabs["UI"] = (t <= s)
    tabs["ones"] = np.ones((128, 128), bool)
    tabs["D16"] = (t // 16 == s // 16) & (t > s)
    tabs["D16T"] = tabs["D16"].T
    for b in (16, 32, 64):
        em = (t // (2 * b) == s // (2 * b)) & ((t % (2 * b)) >= b) & ((s % (2 * b)) < b)
        tabs[f"E{b}"] = em
        tabs[f"F{b}"] = em.T
    names = list(tabs)
    arr = np.concatenate([tabs[n].astype(np.float32) for n in names], axis=1)
    return names, np.ascontiguousarray(arr)


CNAMES, CARR = _consts()


def build_program(debug=None):
    nc = bass.Bass("TRN2", target_bir_lowering=False)

    def din(name, shape):
        return nc.dram_tensor(name, list(shape), F32, kind="ExternalInput").ap()

    xT_d = din("xT", (D, L))
    x_d = din("x", (L, D))
    w_in = din("w_in", (D, PROJ))
    convT = din("convT", (128, 24 * 4))
    a_log = din("a_log", (1, NH))
    dt_bias = din("dt_bias", (1, NH))
    dn_norm_g = din("dn_norm_g", (1, 128))
    w_dn_out = din("w_dn_out", (D, D))
    sgu_ln_g = din("sgu_ln_g", (1, D))
    sgu_ln_b = din("sgu_ln_b", (1, D))
    spwT = din("spwT", (8, 128, 128))
    spb = din("spb", (1, 8 * 128))
    w_sgu_out = din("w_sgu_out", (D, D))
    w_out = din("w_out", (D, D))
    ln1_g = din("ln1_g", (1, D))
    ln1_b = din("ln1_b", (1, D))
    w_rt = din("w_rt", (D, 20))
    b_rt = din("b_rt", (1, 20))
    ew_gate = din("ew_gate", (NE, D, FF))
    ew_up = din("ew_up", (NE, D, FF))
    ew_down = din("ew_down", (NE, FF, D))
    ln2_g = din("ln2_g", (1, D))
    ln2_b = din("ln2_b", (1, D))
    ctab = din("ctab", (128, CARR.shape[1]))
    out_d = nc.dram_tensor("out", [L, D], F32, kind="ExternalOutput").ap()
    h1_d = nc.dram_tensor("h1s", [L, D], F32, kind="Internal").ap()
    dbg = {}
    if debug:
        for name, shape in debug.items():
            dbg[name] = nc.dram_tensor("dbg_" + name, list(shape), F32, kind="ExternalOutput").ap()

    AW = 53200
    with contextlib.ExitStack() as st:
        arena = st.enter_context(nc.sbuf_tensor("arena", [128, AW], F32))
        psum = st.enter_context(nc.psum_tensor("psum", [128, 4096], F32))
        k = KB(nc, arena, AW, psum)
        h1bufs = [Buf(f"h1d{i}") for i in range(NT)]

        def bcast_load(reg, src_row, n, a=0):
            k.dma(reg.f(a, a + n).unsqueeze(1), src_row.partition_broadcast(128), (), [reg.buf], acc=True)

        if os.environ.get('KZERO', '0') == '1':
            zb = Buf('zero')
            for z0 in range(0, AW, 6400):
                k.memset(arena[:, z0:z0 + 6400], 0.0, [zb], eng=DVE if (z0 // 6400) % 2 else POOL)
            k.dead.append(Reg(arena, 0, AW, zb))
        nct = len(CNAMES)
        cf = k.alloc("cf", nct * 128)
        k.dma(cf.f(), ctab, (), [cf.buf])
        cb = k.alloc("cb", nct * 64)
        k.dma(cb.h(), ctab, (), [cb.buf], q=POOL)

        def CF(name):
            j = CNAMES.index(name)
            return cf.f(j * 128, (j + 1) * 128)

        def CBh(name):
            j = CNAMES.index(name)
            return cb.h(j * 128, (j + 1) * 128)

        xT = k.alloc("xT", KC * L // 2)
        xTv = xT.h().rearrange("p (c t) -> p c t", t=L)
        for c in range(KC):
            k.dma(xTv[:, c, :], xT_d[c * 128:(c + 1) * 128, :], (), [xT.buf], q=POOL, acc=True)
        oT = k.alloc("oT", NH * L // 2)
        oTv = oT.h().rearrange("p (h t) -> p h t", t=L)

        mark_dn = k.top
        k.phase(1)
        small = k.alloc("small", 11 * 128)
        sm = lambda j: small.f(j * 128, (j + 1) * 128)
        G_, BETA, EG, EGL, EGLMG, BEG, GG, TMP, TMP2, AB0, AB1 = range(11)
        prm = k.alloc("prm", 16 + 128)
        bcast_load(prm, a_log, NH, 0)
        bcast_load(prm, dt_bias, NH, 8)
        bcast_load(prm, dn_norm_g, 128, 16)
        wab = k.alloc("wab", KC * 16 // 2)
        wabv = wab.h().rearrange("p (c n) -> p c n", n=16)
        k.dma(wabv, w_in[:, C_A:C_A + 16].rearrange("(c p) n -> p c n", p=128), (), [wab.buf], q=POOL)
        abv = small.f(AB0 * 128, AB0 * 128 + 256).rearrange("p (t n) -> p t n", n=16)
        for i in range(NT):
            pb, pbuf = k.bank()
            for c in range(KC):
                k.mm(pb[:, 0:16], xTv[:, c, i * 128:(i + 1) * 128], wabv[:, c, :], c == 0, c == KC - 1,
                     [xT.buf, wab.buf], [pbuf])
            k.act(abv[:, i, :], pb[:, 0:16], AF.Copy, [pbuf], [small.buf])
        a_v = abv[:, :, 0:8]
        b_v = abv[:, :, 8:16]
        v3 = lambda j: sm(j).rearrange("p (t n) -> p t n", n=8)
        sb = [small.buf]
        k.tt(v3(TMP), a_v, prm.f(8, 16).unsqueeze(1).to_broadcast([128, NT, 8]), ALU.add, sb + [prm.buf], sb)
        k.act(sm(TMP), sm(TMP), AF.Exp, sb, sb)
        k.ts(sm(TMP), sm(TMP), 1.0, None, ALU.add, None, sb, sb)
        k.act(sm(TMP), sm(TMP), AF.Ln, sb, sb)
        k.act(prm.f(0, 8), prm.f(0, 8), AF.Exp, [prm.buf], [prm.buf])
        k.stt(v3(GG), v3(TMP), -1.0, prm.f(0, 8).unsqueeze(1).to_broadcast([128, NT, 8]), ALU.mult, ALU.mult,
              sb + [prm.buf], sb)
        k.act(v3(BETA), b_v, AF.Exp, sb, sb, scale=-1.0)
        k.ts(sm(BETA), sm(BETA), 1.0, None, ALU.add, None, sb, sb)
        k.generic(DVE, lambda e: e.reciprocal(out=sm(BETA), in_=sm(BETA)), sb, sb)
        pb, pbuf = k.bank()
        k.mm(pb[:, 0:128], CF("U"), sm(GG), True, True, [cf.buf] + sb, [pbuf])
        k.mm(pb[:, 128:256], CF("ones"), sm(GG), True, True, [cf.buf] + sb, [pbuf])
        k.act(sm(G_), pb[:, 0:128], AF.Copy, [pbuf], sb)
        k.act(sm(EG), pb[:, 0:128], AF.Exp, [pbuf], sb)
        k.act(sm(EGL), pb[:, 128:256], AF.Exp, [pbuf], sb)
        k.tt(sm(TMP2), pb[:, 128:256], sm(G_), ALU.subtract, [pbuf] + sb, sb)
        k.act(sm(EGLMG), sm(TMP2), AF.Exp, sb, sb)
        k.tt(sm(BEG), sm(BETA), sm(EG), ALU.mult, sb, sb)
        k.ts(prm.f(16, 144), prm.f(16, 144), float(np.sqrt(128.0)), None, ALU.mult, None, [prm.buf], [prm.buf])

        if os.environ.get('KNAN'):
            nn = k.alloc('nantest', 8)
            k.memset(nn.f(), -1.0, [nn.buf])
            k.act(nn.f(), nn.f(), AF.Ln, [nn.buf], [nn.buf])
            k.kill([nn])
            k.top = nn.off
        k.phase(2)
        cw = k.alloc("cw", 96)
        k.dma(cw.f(), convT, (), [cw.buf])
        cinL = [k.alloc(f"cin{j}", (4 + L + 12) // 2) for j in range(2)]
        dgL = [k.alloc(f"dg{j}", 4 * 64) for j in range(2)]
        caccL = [k.alloc(f"cacc{j}", L) for j in range(2)]
        csqL = [k.alloc(f"csq{j}", L // 2) for j in range(2)]
        _crn = k.alloc("crn", 512)
        crnL = [_crn, _crn]
        for cin_ in cinL:
            k.memset(cin_.h(0, 4), 0.0, [cin_.buf])
        _wp = k.alloc("wp", 4 * KC * 256 // 2)
        wp = [_wp, _wp]
        qkv = k.alloc("qkvT", 6 * L // 2)
        qkvv = qkv.h().rearrange("p (c t) -> p c t", t=L)
        szr = k.alloc("sz", NT * 256 // 2)
        szv = szr.h().rearrange("p (t n) -> p t n", n=256)
        S32 = k.alloc("S32", 2 * 128)
        Sbf = k.alloc("Sbf", 2 * 64)
        NU = 4
        UNIT_REGS = (("tA", 64), ("tB", 64), ("tA1", 64), ("tB1", 64), ("tA2", 128), ("tE0", 64), ("tE1", 64),
                     ("tE2", 64), ("tF0", 64), ("tF1", 64), ("tR", 64), ("tT", 64), ("tZ", 128), ("tD", 256),
                     ("tX", 256), ("tGUh", 128), ("tkbd", 64), ("tvb", 64))
        HAND_REGS = (("tqk", 64), ("tkdec", 64), ("tu", 128), ("tWT", 64))
        UT = [dict() for _ in range(NU)]
        for nm, n in UNIT_REGS:
            for u in range(NU):
                UT[u][nm] = k.alloc(f"{nm}_{u}", n)

        HT = [[{nm: k.alloc(f"{nm}_h{par}_{u}", n) for nm, n in HAND_REGS} for u in range(NU)] for par in range(2)]

        def gview(nm):
            r0 = UT[0][nm]
            return arena[:, r0.off:r0.off + NU * r0.n].bitcast(BF16).rearrange("p (u c) -> p u c", c=128)

        def gbufs(nm):
            return [UT[u][nm].buf for u in range(NU)]
        PT = [{nm: k.alloc(f"{nm}_p{u}", n) for nm, n in (("tvn", 64), ("to1", 128), ("to", 128), ("tof", 64), ("tss", 8))}
              for u in range(2)]

        def load_pair_weights(p, wr):
            wv = wr.h().rearrange("p (j c n) -> p j c n", j=4, n=256)
            for j, c0 in enumerate((C_QKV + p * 256, C_QKV + 1024 + p * 256, C_QKV + 2048 + p * 256, C_Z + p * 256)):
                k.dma(wv[:, j], w_in[:, c0:c0 + 256].rearrange("(c p) n -> p c n", p=128), (), [wr.buf], q=POOL, acc=True)
            return wv

        for p in range(int(os.environ.get('KPAIRS', '4'))):
            k.phase(2.1)
            wr = wp[0]
            wv = wv_next if p > 0 else load_pair_weights(0, wr)
            identb_ = CBh("ident")
            for ct in range(6):
                j, hh = ct // 2, ct % 2
                gct = j * 8 + 2 * p + hh
                cin, cacc, csq = cinL[ct % 2], caccL[ct % 2], csqL[ct % 2]
                dg = dgL[ct % 2]
                for tap in range(4):
                    k.ts(dg.h(tap * 128, (tap + 1) * 128), identb_, cw.f(gct * 4 + tap, gct * 4 + tap + 1), None, ALU.mult, None,
                         [cb.buf, cw.buf], [dg.buf])
                for tb in range(4):
                    pb, pbuf = k.bank()
                    for c in range(KC):
                        k.mm(pb, wv[:, j, c, hh * 128:(hh + 1) * 128], xTv[:, c, tb * 512:(tb + 1) * 512],
                             c == 0, c == KC - 1, [wr.buf, xT.buf], [pbuf])
                    k.act(cin.h(4 + tb * 512, 4 + (tb + 1) * 512), pb, AF.Copy, [pbuf], [cin.buf])
                for tb in range(4):
                    pb, pbuf = k.bank()
                    for tap in range(4):
                        k.mm(pb, dg.h(tap * 128, (tap + 1) * 128), cin.h(1 + tb * 512 + tap, 1 + tb * 512 + tap + 512),
                             tap == 0, tap == 3, [dg.buf, cin.buf], [pbuf])
                    if j == 2:
                        k.act(qkvv[:, ct, tb * 512:(tb + 1) * 512], pb, AF.Silu, [pbuf], [qkv.buf])
                    else:
                        k.act(cacc.f(tb * 512, (tb + 1) * 512), pb, AF.Silu, [pbuf], [cacc.buf])
                if j != 2:
                    k.act(csq.h(), cacc.f(), AF.Square, [cacc.buf], [csq.buf])
                    for tb in range(4):
                        pb, pbuf = k.bank()
                        crn = crnL[tb % 2]
                        k.mm(pb, CBh("ones"), csq.h(tb * 512, (tb + 1) * 512), True, True, [cb.buf, csq.buf], [pbuf])
                        k.rsqrt(crn.f(), pb, NORM_EPS, [pbuf], [crn.buf])
                        k.stt(qkvv[:, ct, tb * 512:(tb + 1) * 512], cacc.f(tb * 512, (tb + 1) * 512),
                              (128.0 ** -0.5) if j == 0 else 1.0, crn.f(), ALU.mult, ALU.mult,
                              [cacc.buf, crn.buf], [qkv.buf])
            k.phase(2.2)
            for i in range(NT):
                pb, pbuf = k.bank()
                for c in range(KC):
                    k.mm(pb[:, 0:256], xTv[:, c, i * 128:(i + 1) * 128], wv[:, 3, c, :], c == 0, c == KC - 1,
                         [xT.buf, wr.buf], [pbuf])
                k.act(szv[:, i, :], pb[:, 0:256], AF.Silu, [pbuf], [szr.buf])
            if p < 3:
                wv_next = load_pair_weights(p + 1, wr)
            k.memset(S32.f(), 0.0, [S32.buf])
            k.memset(Sbf.h(), 0.0, [Sbf.buf])
            identb = CBh("ident")

            def pre(i, hh, T):
                ts_ = slice(i * 128, (i + 1) * 128)
                h = 2 * p + hh
                col = i * 8 + h
                sc = lambda j: sm(j)[:, col:col + 1]
                qT = qkvv[:, 0 + hh, ts_]
                kT = qkvv[:, 2 + hh, ts_]
                vT = qkvv[:, 4 + hh, ts_]
                tA, tB, tA1, tB1, tA2, tR, tT, tZ, tD, tX = (T[n] for n in ("tA", "tB", "tA1", "tB1", "tA2", "tR", "tT", "tZ", "tD", "tX"))
                tGUh, tqk, tkbd, tkdec, tvb, tu, tWT = (T[n] for n in ("tGUh", "tqk", "tkbd", "tkdec", "tvb", "tu", "tWT"))
                tE = [T["tE0"], T["tE1"], T["tE2"]]
                tF = [T["tF0"], T["tF1"]]
                pb, pbuf = k.bank()
                k.tr(pb[:, 0:128], kT, identb, [qkv.buf, cb.buf], [pbuf])
                k.tr(pb[:, 128:256], vT, identb, [qkv.buf, cb.buf], [pbuf])
                k.act(tkbd.h(), pb[:, 0:128], AF.Identity, [pbuf, small.buf], [tkbd.buf], scale=sc(BEG))
                k.act(tvb.h(), pb[:, 128:256], AF.Identity, [pbuf, small.buf], [tvb.buf], scale=sc(BETA))
                k.ts(tkdec.h(), pb[:, 0:128], sc(EGLMG), None, ALU.mult, None, [pbuf, small.buf], [tkdec.buf])
                k.act(tGUh.h(0, 128), CF("U"), AF.Identity, [cf.buf, small.buf], [tGUh.buf], scale=sc(GG))
                k.stt(tGUh.h(128, 256), CF("U"), sc(GG), tGUh.h(0, 128), ALU.mult, ALU.subtract,
                      [cf.buf, small.buf, tGUh.buf], [tGUh.buf])
                yield
                pb, pbuf = k.bank()
                k.mm(pb[:, 0:128], kT, kT, True, True, [qkv.buf], [pbuf])
                k.mm(pb[:, 128:256], kT, qT, True, True, [qkv.buf], [pbuf])
                k.mm(pb[:, 256:384], tGUh.h(0, 128), CBh("SL"), True, False, [tGUh.buf, cb.buf], [pbuf])
                k.mm(pb[:, 256:384], tGUh.h(128, 256), CBh("SL"), False, True, [tGUh.buf, cb.buf], [pbuf])
                k.mm(pb[:, 384:512], CBh("SL"), tGUh.h(0, 128), True, False, [tGUh.buf, cb.buf], [pbuf])
                k.mm(pb[:, 384:512], CBh("SL"), tGUh.h(128, 256), False, True, [tGUh.buf, cb.buf], [pbuf])
                k.act(tD.f(), pb[:, 256:512], AF.Exp, [pbuf], [tD.buf])
                k.tt(tX.f(), tD.f(), pb[:, 0:256], ALU.mult, [tD.buf, pbuf], [tX.buf])
                k.stt(tA.h(), tX.f(0, 128), sc(BETA), CF("SL"), ALU.mult, ALU.mult,
                      [tX.buf, small.buf, cf.buf], [tA.buf])
                k.tt(tqk.h(), tX.f(128, 256), CF("UI"), ALU.mult, [tX.buf, cf.buf], [tqk.buf])
                yield
                pb, pbuf = k.bank()
                k.tr(pb[:, 0:128], tA.h(), identb, [tA.buf, cb.buf], [pbuf])
                k.act(tB.h(), pb[:, 0:128], AF.Copy, [pbuf], [tB.buf])
                yield

            def group_masks():
                def bc(name):
                    return CBh(name).unsqueeze(1).to_broadcast([128, NU, 128])
                k.tt(gview("tA1"), gview("tA"), bc("D16"), ALU.mult, gbufs("tA") + [cb.buf], gbufs("tA1"))
                k.tt(gview("tB1"), gview("tB"), bc("D16T"), ALU.mult, gbufs("tB") + [cb.buf], gbufs("tB1"))
                k.tt(gview("tR"), bc("ident"), gview("tB1"), ALU.subtract, gbufs("tB1") + [cb.buf], gbufs("tR"))
                k.tt(gview("tT"), bc("ident"), gview("tA1"), ALU.subtract, gbufs("tA1") + [cb.buf], gbufs("tT"))
                for li, b in enumerate((16, 32, 64)):
                    k.tt(gview(f"tE{li}"), gview("tA"), bc(f"E{b}"), ALU.mult, gbufs("tA") + [cb.buf], gbufs(f"tE{li}"))
                    if b < 64:
                        k.tt(gview(f"tF{li}"), gview("tB"), bc(f"F{b}"), ALU.mult, gbufs("tB") + [cb.buf], gbufs(f"tF{li}"))

            def pre_b(i, hh, T):
                tA, tB, tA1, tB1, tA2, tR, tT, tZ = (T[n] for n in ("tA", "tB", "tA1", "tB1", "tA2", "tR", "tT", "tZ"))
                tkbd, tvb, tu, tWT = (T[n] for n in ("tkbd", "tvb", "tu", "tWT"))
                tE = [T["tE0"], T["tE1"], T["tE2"]]
                tF = [T["tF0"], T["tF1"]]
                curA, curB, curAb, curBb = tA1.h(), tB1.h(), tA1.buf, tB1.buf
                for lev in range(3):
                    pb, pbuf = k.bank()
                    k.mm(pb[:, 0:128], curB, curA, True, True, [curAb, curBb], [pbuf])
                    k.mm(pb[:, 128:256], curA, curB, True, True, [curAb, curBb], [pbuf])
                    k.act(tA2.h(), pb[:, 0:256], AF.Copy, [pbuf], [tA2.buf])
                    yield
                    nA, nB = tA2.h(0, 128), tA2.h(128, 256)
                    pb2, pbuf2 = k.bank()
                    k.mm(pb2[:, 0:128], nA, tR.h(), True, True, [tA2.buf, tR.buf], [pbuf2])
                    k.mm(pb2[:, 128:256], nB, tT.h(), True, True, [tA2.buf, tT.buf], [pbuf2])
                    k.tt(tR.h(), tR.h(), pb2[:, 0:128], ALU.add, [tR.buf, pbuf2], [tR.buf])
                    k.tt(tT.h(), tT.h(), pb2[:, 128:256], ALU.add, [tT.buf, pbuf2], [tT.buf])
                    curA, curB, curAb, curBb = tA2.h(0, 128), tA2.h(128, 256), tA2.buf, tA2.buf
                    yield
                for li, b in enumerate((16, 32, 64)):
                    pb, pbuf = k.bank()
                    k.mm(pb[:, 0:128], tE[li].h(), tR.h(), True, True, [tE[li].buf, tR.buf], [pbuf])
                    if b < 64:
                        k.mm(pb[:, 128:256], tF[li].h(), tT.h(), True, True, [tF[li].buf, tT.buf], [pbuf])
                        k.act(tZ.h(), pb[:, 0:256], AF.Copy, [pbuf], [tZ.buf])
                    else:
                        k.act(tZ.h(0, 128), pb[:, 0:128], AF.Copy, [pbuf], [tZ.buf])
                    yield
                    pb2, pbuf2 = k.bank()
                    k.mm(pb2[:, 0:128], tT.h(), tZ.h(0, 128), True, True, [tT.buf, tZ.buf], [pbuf2])
                    if b < 64:
                        k.mm(pb2[:, 128:256], tR.h(), tZ.h(128, 256), True, True, [tR.buf, tZ.buf], [pbuf2])
                    k.tt(tR.h(), tR.h(), pb2[:, 0:128], ALU.subtract, [tR.buf, pbuf2], [tR.buf])
                    if b < 64:
                        k.tt(tT.h(), tT.h(), pb2[:, 128:256], ALU.subtract, [tT.buf, pbuf2], [tT.buf])
                    yield
                pb, pbuf = k.bank()
                k.mm(pb[:, 0:128], tR.h(), tvb.h(), True, True, [tR.buf, tvb.buf], [pbuf])
                k.mm(pb[:, 128:256], tkbd.h(), tR.h(), True, True, [tR.buf, tkbd.buf], [pbuf])
                k.act(tu.f(), pb[:, 0:128], AF.Copy, [pbuf], [tu.buf])
                k.act(tWT.h(), pb[:, 128:256], AF.Copy, [pbuf], [tWT.buf])
                yield

            def post(i, hh, T, P):
                ts_ = slice(i * 128, (i + 1) * 128)
                h = 2 * p + hh
                col = i * 8 + h
                sc = lambda j: sm(j)[:, col:col + 1]
                qT = qkvv[:, 0 + hh, ts_]
                tqk, tkdec, tu, tWT = T["tqk"], T["tkdec"], T["tu"], T["tWT"]
                tvn, to1, to, tof, tss = P["tvn"], P["to1"], P["to"], P["tof"], P["tss"]
                Sh = Sbf.h(hh * 128, (hh + 1) * 128)
                Sf = S32.f(hh * 128, (hh + 1) * 128)
                pb, pbuf = k.bank()
                k.mm(pb[:, 0:128], tWT.h(), Sh, True, True, [tWT.buf, Sbf.buf], [pbuf])
                k.mm(pb[:, 128:256], qT, Sh, True, True, [qkv.buf, Sbf.buf], [pbuf])
                k.tt(tvn.h(), tu.f(), pb[:, 0:128], ALU.subtract, [tu.buf, pbuf], [tvn.buf])
                k.act(to1.f(), pb[:, 128:256], AF.Identity, [pbuf, small.buf], [to1.buf], scale=sc(EG))
                yield
                pb2, pbuf2 = k.bank()
                k.mm(pb2[:, 0:128], tqk.h(), tvn.h(), True, True, [tqk.buf, tvn.buf], [pbuf2])
                k.mm(pb2[:, 128:256], tkdec.h(), tvn.h(), True, True, [tkdec.buf, tvn.buf], [pbuf2])
                k.stt(Sf, Sf, sc(EGL), pb2[:, 128:256], ALU.mult, ALU.add, [S32.buf, small.buf, pbuf2], [S32.buf])
                k.act(Sh, Sf, AF.Copy, [S32.buf], [Sbf.buf])
                k.tt(to.f(), to1.f(), pb2[:, 0:128], ALU.add, [to1.buf, pbuf2], [to.buf])
                yield
                k.act(to1.f(), to.f(), AF.Square, [to.buf, tss.buf], [to1.buf, tss.buf], accum=tss.f(0, 1))
                k.rsqrt(tss.f(1, 2), tss.f(0, 1), 128.0 * NORM_EPS, [tss.buf], [tss.buf])
                k.stt(to.f(), to.f(), tss.f(1, 2), prm.f(16, 144), ALU.mult, ALU.mult,
                      [to.buf, tss.buf, prm.buf], [to.buf])
                k.tt(tof.h(), to.f(), szv[:, i, hh * 128:(hh + 1) * 128], ALU.mult, [to.buf, szr.buf], [tof.buf])
                yield
                pb, pbuf = k.bank()
                k.tr(pb[:, 0:128], tof.h(), identb, [tof.buf, cb.buf], [pbuf])
                k.act(oTv[:, h, ts_], pb[:, 0:128], AF.Copy, [pbuf], [oT.buf])
                yield

            def lockstep(gens):
                gens = list(gens)
                while gens:
                    nxt = []
                    for g_ in gens:
                        try:
                            next(g_)
                            nxt.append(g_)
                        except StopIteration:
                            pass
                    gens = nxt

            TG = NU // 2
            groups = list(range(0, NT, TG))

            def Tm(gi, u):
                d = dict(UT[u])
                d.update(HT[gi % 2][u])
                return d

            def step(gens):
                alive = []
                for g_ in gens:
                    try:
                        next(g_)
                        alive.append(g_)
                    except StopIteration:
                        pass
                return alive

            def pre_group(gi):
                g0 = groups[gi]
                units = [(i, hh) for i in range(g0, g0 + TG) for hh in range(2)]
                gens = [pre(i, hh, Tm(gi, u)) for u, (i, hh) in enumerate(units)]
                while gens:
                    gens = step(gens)
                    yield
                group_masks()
                yield
                gens = [pre_b(i, hh, Tm(gi, u)) for u, (i, hh) in enumerate(units)]
                while gens:
                    gens = step(gens)
                    yield

            def post_group(gi):
                g0 = groups[gi]
                for i in range(g0, g0 + TG):
                    gens = [post(i, hh, Tm(gi, (i - g0) * 2 + hh), PT[hh]) for hh in range(2)]
                    while gens:
                        gens = step(gens)
                        yield

            def run2(a_, b_):
                threads = [t for t in (a_, b_) if t is not None]
                while threads:
                    threads = step(threads)

            run2(pre_group(0), None)
            for gi in range(len(groups)):
                run2(post_group(gi), pre_group(gi + 1) if gi + 1 < len(groups) else None)

        if debug and "oT" in dbg:
            dt_ = k.alloc("dbgt", NH * L)
            k.cp(dt_.f(), oT.h(), [oT.buf], [dt_.buf])
            k.dma(dbg["oT"], dt_.f(), [dt_.buf], [])

        dn_regs = [small, prm, wab, cw, wp[0], qkv, szr, S32, Sbf] + cinL + caccL + csqL + [crnL[0]] + dgL
        for T_ in UT + PT + HT[0] + HT[1]:
            dn_regs += list(T_.values())
        k.kill(dn_regs)
        k.top = mark_dn

        k.phase(3)
        def layer_norm(xin, xbuf, gam, bet, pbufs, outs, obufs, stats):
            st6 = stats.f(0, 12).rearrange("p (a b) -> p a b", b=6)
            k.generic(DVE, lambda e: e.bn_stats(out=st6[:, 0, :], in_=xin[:, 0:512]), [xbuf], [stats.buf])
            k.generic(DVE, lambda e: e.bn_stats(out=st6[:, 1, :], in_=xin[:, 512:1024]), [xbuf], [stats.buf])
            k.generic(DVE, lambda e: e.bn_aggr(out=stats.f(12, 14), in_=stats.f(0, 12)), [stats.buf], [stats.buf])
            k.rsqrt(stats.f(14, 15), stats.f(13, 14), LN_EPS, [stats.buf], [stats.buf])
            k.ts(xin, xin, stats.f(12, 13), stats.f(14, 15), ALU.subtract, ALU.mult, [xbuf, stats.buf], [xbuf])
            k.tt(xin, xin, gam, ALU.mult, [xbuf] + pbufs, [xbuf])
            for o, ob in zip(outs, obufs):
                k.tt(o, xin, bet, ALU.add, [xbuf] + pbufs, [ob])

        lnp = k.alloc("lnp", 2 * D + 1024)
        SK = os.environ.get('KSKIP', '')
        if 'b' not in SK:
            bcast_load(lnp, sgu_ln_g, D, 0)
            bcast_load(lnp, sgu_ln_b, D, D)
            bcast_load(lnp, spb, 1024, 2 * D)
        wsp = k.alloc("wsp", 8 * 128)
        wspv = wsp.f().rearrange("p (g t) -> p g t", t=128)
        if 'a' not in SK:
            k.dma(wspv, spwT.rearrange("g s t -> s g t"), (), [wsp.buf])
        wspb = k.alloc("wspb", 8 * 64)
        wspbv = wspb.h().rearrange("p (g t) -> p g t", t=128)
        if 'c' not in SK:
            k.tt(wspbv, wspv, CF("UI").unsqueeze(1).to_broadcast([128, 8, 128]), ALU.mult, [wsp.buf, cf.buf], [wspb.buf])
        gT = k.alloc("gT", KC * L // 2)
        gTv = gT.h().rearrange("p (c t) -> p c t", t=L)
        wblk = [k.alloc(f"wblk{j}", KC * 512 // 2) for j in range(2)]
        vtmp = k.alloc("vtmp", D)
        stt_ = k.alloc("stats", 16)
        mark_vn = k.top
        vn = k.alloc("vn", NT * D // 2)
        vnv = vn.h().rearrange("p (t n) -> p t n", n=D)
        k.phase(3.1)
        nblk = 0
        for half in range(2):
            wr = wblk[nblk % 2]
            nblk += 1
            wvv = wr.h().rearrange("p (c n) -> p c n", n=512)
            c0 = C_UV + D + half * 512
            k.dma(wvv, w_in[:, c0:c0 + 512].rearrange("(c p) n -> p c n", p=128), (), [wr.buf], q=POOL)
            for i in range(NT):
                pb, pbuf = k.bank()
                for c in range(KC):
                    k.mm(pb, xTv[:, c, i * 128:(i + 1) * 128], wvv[:, c, :], c == 0, c == KC - 1, [xT.buf, wr.buf], [pbuf])
                k.act(vnv[:, i, half * 512:(half + 1) * 512], pb, AF.Gelu, [pbuf], [vn.buf])
        k.phase(3.2)
        for i in range(NT):
            k.act(vtmp.f(), vnv[:, i, :], AF.Copy, [vn.buf], [vtmp.buf])
            layer_norm(vtmp.f(), vtmp.buf, lnp.f(0, D), lnp.f(D, 2 * D), [lnp.buf], [vnv[:, i, :]], [vn.buf], stt_)
        k.phase(3.3)
        for half in range(2):
            wr = wblk[nblk % 2]
            nblk += 1
            wvv = wr.h().rearrange("p (c n) -> p c n", n=512)
            c0 = C_UV + half * 512
            k.dma(wvv, w_in[:, c0:c0 + 512].rearrange("(c p) n -> p c n", p=128), (), [wr.buf], q=POOL)
            for cc in range(4):
                for tb in range(4):
                    pb, pbuf = k.bank()
                    for c in range(KC):
                        k.mm(pb, wvv[:, c, cc * 128:(cc + 1) * 128], xTv[:, c, tb * 512:(tb + 1) * 512],
                             c == 0, c == KC - 1, [xT.buf, wr.buf], [pbuf])
                    k.act(gTv[:, half * 4 + cc, tb * 512:(tb + 1) * 512], pb, AF.Gelu, [pbuf], [gT.buf])
        k.phase(3.4)
        for i in range(NT):
            for gh in range(2):
                pb, pbuf = k.bank()
                for gq in range(4):
                    g = gh * 4 + gq
                    k.mm(pb[:, gq * 128:(gq + 1) * 128], vnv[:, i, g * 128:(g + 1) * 128], wspbv[:, g, :], True, True,
                         [vn.buf, wspb.buf], [pbuf])
                tmpv = vtmp.f(0, 512).rearrange("p (g t) -> p g t", t=128)
                k.tt(tmpv, pb.rearrange("p (g t) -> p g t", t=128),
                     lnp.f(2 * D + gh * 512, 2 * D + (gh + 1) * 512).rearrange("p (g t) -> p g t", t=128), ALU.add,
                     [pbuf, lnp.buf], [vtmp.buf])
                k.tt(gTv[:, gh * 4:(gh + 1) * 4, i * 128:(i + 1) * 128], gTv[:, gh * 4:(gh + 1) * 4, i * 128:(i + 1) * 128],
                     tmpv, ALU.mult, [gT.buf, vtmp.buf], [gT.buf])
        k.kill([vn, wblk[0], wblk[1], vtmp, stt_, lnp, wsp, wspb])
        k.top = mark_vn
        k.phase(4)
        yT = k.alloc("yT", KC * L // 2)
        yTv = yT.h().rearrange("p (c t) -> p c t", t=L)
        wm = [k.alloc(f"wm{j}", 4 * KC * 128 // 2) for j in range(2)]
        sg = [k.alloc(f"sg{j}", 512) for j in range(2)]
        for dc in range(KC):
            wr = wm[dc % 2]
            wmv = wr.h().rearrange("p (j c n) -> p j c n", j=4, n=128)
            srcs = (w_dn_out[:, dc * 128:(dc + 1) * 128], w_sgu_out[:, dc * 128:(dc + 1) * 128],
                    w_in[:, C_GDN + dc * 128:C_GDN + (dc + 1) * 128], w_in[:, C_GSGU + dc * 128:C_GSGU + (dc + 1) * 128])
            for j, s_ in enumerate(srcs):
                k.dma(wmv[:, j], s_.rearrange("(c p) n -> p c n", p=128), (), [wr.buf], q=POOL, acc=True)
            for tb in range(4):
                tsl = slice(tb * 512, (tb + 1) * 512)
                banks = [k.bank() for _ in range(4)]
                rhs_src = (oTv, gTv, xTv, xTv)
                rbufs = (oT.buf, gT.buf, xT.buf, xT.buf)
                for j in range(4):
                    pb, pbuf = banks[j]
                    for c in range(KC):
                        k.mm(pb, wmv[:, j, c, :], rhs_src[j][:, c, tsl], c == 0, c == KC - 1, [wr.buf, rbufs[j]], [pbuf])
                k.act(sg[0].f(), banks[2][0], AF.Sigmoid, [banks[2][1]], [sg[0].buf])
                k.act(sg[1].f(), banks[3][0], AF.Sigmoid, [banks[3][1]], [sg[1].buf])
                k.tt(sg[0].f(), sg[0].f(), banks[0][0], ALU.mult, [sg[0].buf, banks[0][1]], [sg[0].buf])
                k.tt(sg[1].f(), sg[1].f(), banks[1][0], ALU.mult, [sg[1].buf, banks[1][1]], [sg[1].buf])
                k.tt(yTv[:, dc, tsl], sg[0].f(), sg[1].f(), ALU.add, [sg[0].buf, sg[1].buf], [yT.buf])

        k.phase(5)
        k.kill([xT, oT, gT, wm[0], wm[1], sg[0], sg[1]])
        k.top = cb.off + cb.n
        comb = k.alloc("comb", NT * NE)
        combv = comb.f().rearrange("p (t e) -> p t e", e=NE)
        h1T = k.alloc("h1T", KC * L // 2)
        h1Tv = h1T.h().rearrange("p (c t) -> p c t", t=L)
        mark5 = k.top
        ln1p = k.alloc("ln1p", 2 * D)
        bcast_load(ln1p, ln1_g, D, 0)
        bcast_load(ln1p, ln1_b, D, D)
        wo = k.alloc("wo", KC * D // 2)
        wov = wo.h().rearrange("p (c n) -> p c n", n=D)
        k.dma(wov, w_out.rearrange("(c p) n -> p c n", p=128), (), [wo.buf], q=POOL)
        wrt = k.alloc("wrt", KC * 20 + 24)
        wrtv = wrt.f(0, KC * 20).rearrange("p (c n) -> p c n", n=20)
        k.dma(wrtv, w_rt.rearrange("(c p) n -> p c n", p=128), (), [wrt.buf])
        bcast_load(wrt, b_rt, 20, KC * 20)
        xres = [k.alloc(f"xres{j}", D) for j in range(2)]
        h1t = [k.alloc(f"h1t{j}", D) for j in range(2)]
        h1Tf = k.alloc("h1Tf", KC * 128)
        h1Tfv = h1Tf.f().rearrange("p (c t) -> p c t", t=128)
        stat1 = k.alloc("stat1", 16)
        rt = k.alloc("rt", 256)
        identf = CF("ident")
        for i in range(NT):
            xr, hb = xres[i % 2], h1t[i % 2]
            k.dma(xr.f(), x_d[i * 128:(i + 1) * 128, :], (), [xr.buf])
            for half in range(2):
                pb, pbuf = k.bank()
                for c in range(KC):
                    k.mm(pb, yTv[:, c, i * 128:(i + 1) * 128], wov[:, c, half * 512:(half + 1) * 512], c == 0, c == KC - 1,
                         [yT.buf, wo.buf], [pbuf])
                k.stt(hb.f(half * 512, (half + 1) * 512), xr.f(half * 512, (half + 1) * 512), ALPHA, pb, ALU.mult, ALU.add,
                      [xr.buf, pbuf], [hb.buf])
            layer_norm(hb.f(), hb.buf, ln1p.f(0, D), ln1p.f(D, 2 * D), [ln1p.buf], [hb.f()], [hb.buf], stat1)
            k.dma(h1_d[i * 128:(i + 1) * 128, :], hb.f(), [hb.buf], [h1bufs[i]])
            for c4 in range(2):
                pb, pbuf = k.bank()
                for cc in range(4):
                    c = c4 * 4 + cc
                    k.tr(pb[:, cc * 128:(cc + 1) * 128], hb.f(c * 128, (c + 1) * 128), identf, [hb.buf, cf.buf], [pbuf])
                k.act(h1Tv[:, c4 * 4:(c4 + 1) * 4, i * 128:(i + 1) * 128], pb.rearrange("p (c t) -> p c t", t=128), AF.Copy,
                      [pbuf], [h1T.buf])
                k.cp(h1Tfv[:, c4 * 4:(c4 + 1) * 4, :], pb.rearrange("p (c t) -> p c t", t=128), [pbuf], [h1Tf.buf])
            pb, pbuf = k.bank()
            for c in range(KC):
                k.mm(pb[:, 0:20], h1Tfv[:, c, :], wrtv[:, c, :], c == 0, c == KC - 1, [h1Tf.buf, wrt.buf], [pbuf])
            R_ = lambda a, b: rt.f(a, b)
            rb = [rt.buf]
            k.tt(R_(0, 20), pb[:, 0:20], wrt.f(KC * 20, KC * 20 + 20), ALU.add, [pbuf, wrt.buf], rb)
            k.generic(DVE, lambda e: e.reduce_max(out=R_(20, 21), in_=R_(0, 4), axis=AX.X), rb, rb)
            k.ts(R_(24, 28), R_(0, 4), R_(20, 21), None, ALU.is_equal, None, rb, rb)
            k.ts(R_(28, 32), R_(0, 4), R_(20, 21), None, ALU.subtract, None, rb, rb)
            k.memset(R_(21, 22), 0.0, rb)
            k.act(R_(28, 32), R_(28, 32), AF.Exp, rb, rb, accum=R_(21, 22))
            k.generic(DVE, lambda e: e.reciprocal(out=R_(22, 23), in_=R_(21, 22)), rb, rb)
            el = R_(4, 20).rearrange("p (g j) -> p g j", j=4)
            k.tt(R_(32, 48).rearrange("p (g j) -> p g j", j=4), el, R_(24, 28).unsqueeze(2).to_broadcast([128, 4, 4]),
                 ALU.mult, rb, rb)
            k.generic(DVE, lambda e: e.reduce_sum(out=R_(48, 52), in_=R_(32, 48).rearrange("p (g j) -> p j g", j=4),
                                                  axis=AX.X), rb, rb)
            k.generic(DVE, lambda e: e.reduce_max(out=R_(52, 53), in_=R_(48, 52), axis=AX.X), rb, rb)
            k.ts(R_(56, 60), R_(48, 52), R_(52, 53), None, ALU.is_equal, None, rb, rb)
            k.stt(R_(60, 64), R_(56, 60), -1e30, R_(48, 52), ALU.mult, ALU.add, rb, rb)
            k.generic(DVE, lambda e: e.reduce_max(out=R_(53, 54), in_=R_(60, 64), axis=AX.X), rb, rb)
            k.ts(R_(64, 68), R_(60, 64), R_(53, 54), None, ALU.is_equal, None, rb, rb)
            k.tt(R_(54, 55), R_(53, 54), R_(52, 53), ALU.subtract, rb, rb)
            k.act(R_(54, 55), R_(54, 55), AF.Exp, rb, rb)
            k.ts(R_(54, 55), R_(54, 55), 1.0, None, ALU.add, None, rb, rb)
            k.generic(DVE, lambda e: e.reciprocal(out=R_(68, 69), in_=R_(54, 55)), rb, rb)
            k.ts(R_(69, 70), R_(68, 69), -1.0, 1.0, ALU.mult, ALU.add, rb, rb)
            k.tt(R_(68, 70), R_(68, 70), R_(22, 23).to_broadcast([128, 2]), ALU.mult, rb, rb)
            k.ts(R_(72, 76), R_(56, 60), R_(68, 69), None, ALU.mult, None, rb, rb)
            k.stt(R_(72, 76), R_(64, 68), R_(69, 70), R_(72, 76), ALU.mult, ALU.add, rb, rb)
            k.tt(combv[:, i, :].rearrange("p (g j) -> p g j", j=4), R_(24, 28).unsqueeze(2).to_broadcast([128, 4, 4]),
                 R_(72, 76).unsqueeze(1).to_broadcast([128, 4, 4]), ALU.mult, rb, [comb.buf])

        k.phase(6)
        k.kill([yT, ln1p, wo, wrt, xres[0], xres[1], h1t[0], h1t[1], h1Tf, stat1, rt])
        k.top = mark5
        yacc = k.alloc("yacc", NT * D)
        yaccv = yacc.f().rearrange("p (t n) -> p t n", n=D)
        ew = [k.alloc(f"ew{j}", (2 * KC * FF + 2 * D) // 2) for j in range(2)]
        hT = [k.alloc(f"hT{j}", 2 * 512 // 2) for j in range(2)]
        sgl = [k.alloc(f"sgl{j}", 512) for j in range(2)]

        ln2p = k.alloc("ln2p", 2 * D)
        bcast_load(ln2p, ln2_g, D, 0)
        bcast_load(ln2p, ln2_b, D, D)
        hres = [k.alloc(f"hres{j}", D) for j in range(2)]
        stat2 = k.alloc("stat2", 16)
        yb = [Buf(f"yacc{i}") for i in range(NT)]
        for b_ in yb:
            b_.readers = dict(yacc.buf.readers)

        def final_tile(i):
            hr = hres[i % 2]
            k.dma(hr.f(), h1_d[i * 128:(i + 1) * 128, :], [h1bufs[i]], [hr.buf])
            k.stt(hr.f(), hr.f(), ALPHA, yaccv[:, i, :], ALU.mult, ALU.add, [hr.buf, yb[i]], [hr.buf])
            layer_norm(hr.f(), hr.buf, ln2p.f(0, D), ln2p.f(D, 2 * D), [ln2p.buf], [hr.f()], [hr.buf], stat2)
            k.dma(out_d[i * 128:(i + 1) * 128, :], hr.f(), [hr.buf], [])

        def load_expert(e, wr):
            v = wr.h()
            g = v[:, 0:KC * FF].rearrange("p (c n) -> p c n", n=FF)
            u = v[:, KC * FF:2 * KC * FF].rearrange("p (c n) -> p c n", n=FF)
            d = v[:, 2 * KC * FF:2 * KC * FF + 2 * D].rearrange("p (c n) -> p c n", n=D)
            k.dma(g, ew_gate[e].rearrange("(c p) n -> p c n", p=128), (), [wr.buf], q=POOL, acc=True)
            k.dma(u, ew_up[e].rearrange("(c p) n -> p c n", p=128), (), [wr.buf], q=POOL, acc=True)
            k.dma(d, ew_down[e].rearrange("(c p) n -> p c n", p=128), (), [wr.buf], q=POOL, acc=True)
            return g, u, d

        nxt = load_expert(0, ew[0])
        for e in range(NE):
            g_, u_, d_ = nxt
            wr = ew[e % 2]
            if e + 1 < NE:
                nxt = load_expert(e + 1, ew[(e + 1) % 2])
            for tb in range(4):
                tsl = slice(tb * 512, (tb + 1) * 512)
                hr = hT[tb % 2]
                hv = hr.h().rearrange("p (c t) -> p c t", t=512)
                for fc in range(2):
                    pg, pgb = k.bank()
                    pu, pub = k.bank()
                    for c in range(KC):
                        k.mm(pg, g_[:, c, fc * 128:(fc + 1) * 128], h1Tv[:, c, tsl], c == 0, c == KC - 1, [wr.buf, h1T.buf], [pgb])
                    for c in range(KC):
                        k.mm(pu, u_[:, c, fc * 128:(fc + 1) * 128], h1Tv[:, c, tsl], c == 0, c == KC - 1, [wr.buf, h1T.buf], [pub])
                    sl = sgl[fc]
                    k.act(sl.f(), pg, AF.Silu, [pgb], [sl.buf])
                    k.tt(hv[:, fc, :], sl.f(), pu, ALU.mult, [sl.buf, pub], [hr.buf])
                for ti in range(4):
                    i = tb * 4 + ti
                    for half in range(2):
                        pb, pbuf = k.bank()
                        for fc in range(2):
                            k.mm(pb, hv[:, fc, ti * 128:(ti + 1) * 128], d_[:, fc, half * 512:(half + 1) * 512], fc == 0, fc == 1,
                                 [hr.buf, wr.buf], [pbuf])
                        dst = yaccv[:, i, half * 512:(half + 1) * 512]
                        if e == 0:
                            k.ts(dst, pb, combv[:, i, e:e + 1], None, ALU.mult, None, [pbuf, comb.buf], [yb[i]])
                        else:
                            k.stt(dst, pb, combv[:, i, e:e + 1], dst, ALU.mult, ALU.add, [pbuf, comb.buf, yb[i]], [yb[i]])
                    if e == NE - 1:
                        final_tile(i)

        k.enabled = True
        for nm in [x for x in os.environ.get('KDUMP', '').split(',') if x]:
            rg = k.registry[nm]
            dd = nc.dram_tensor('dump_' + nm, [128, rg.n], F32, kind='ExternalOutput').ap()
            k.dma(dd, rg.f(), [rg.buf], [])
        k.B.emit()
        if os.environ.get('KVERBOSE'):
            print('maxwait', {kk: v for kk, v in k.B.maxwait.items() if kk[0] == 'e'})
    return nc


_NC_CACHE = {}


def _prep_inputs(inputs, b):
    f = lambda a: np.ascontiguousarray(np.asarray(a, dtype=np.float32))
    x = np.asarray(inputs["x"], dtype=np.float32)
    m = {}
    m["x"] = f(x[b])
    m["xT"] = f(x[b].T)
    m["w_in"] = f(inputs["w_in"][0])
    cwv = np.asarray(inputs["conv_w"], dtype=np.float32)[0]
    m["convT"] = f(cwv.T.reshape(24, 128, 4).transpose(1, 0, 2).reshape(128, 96))
    m["a_log"] = f(inputs["a_log"]).reshape(1, NH)
    m["dt_bias"] = f(inputs["dt_bias"]).reshape(1, NH)
    m["dn_norm_g"] = f(inputs["dn_norm_g"]).reshape(1, 128)
    m["w_dn_out"] = f(inputs["w_dn_out"][0])
    m["sgu_ln_g"] = f(inputs["sgu_ln_g"]).reshape(1, D)
    m["sgu_ln_b"] = f(inputs["sgu_ln_b"]).reshape(1, D)
    m["spwT"] = f(np.asarray(inputs["spatial_w"], dtype=np.float32)[0].transpose(0, 2, 1))
    m["spb"] = f(inputs["spatial_b"]).reshape(1, 1024)
    m["w_sgu_out"] = f(inputs["w_sgu_out"][0])
    m["w_out"] = f(inputs["w_out"][0])
    m["ln1_g"] = f(inputs["ln1_g"]).reshape(1, D)
    m["ln1_b"] = f(inputs["ln1_b"]).reshape(1, D)
    m["w_rt"] = f(np.concatenate([np.asarray(inputs["router_group_w"], dtype=np.float32)[0],
                                  np.asarray(inputs["router_expert_w"], dtype=np.float32)[0]], axis=1))
    m["b_rt"] = f(np.concatenate([np.asarray(inputs["router_group_b"], dtype=np.float32)[0],
                                  np.asarray(inputs["router_expert_b"], dtype=np.float32)[0]], axis=0)).reshape(1, 20)
    m["ew_gate"] = f(inputs["expert_w_gate"][0])
    m["ew_up"] = f(inputs["expert_w_up"][0])
    m["ew_down"] = f(inputs["expert_w_down"][0])
    m["ln2_g"] = f(inputs["ln2_g"]).reshape(1, D)
    m["ln2_b"] = f(inputs["ln2_b"]).reshape(1, D)
    m["ctab"] = CARR
    return m


def kernel(**inputs):
    if "nc" not in _NC_CACHE:
        _NC_CACHE["nc"] = build_program()
    nc = _NC_CACHE["nc"]
    shared = None
    in_maps = []
    for b in range(8):
        m = _prep_inputs(inputs, b) if shared is None else dict(shared)
        if shared is None:
            shared = m
        else:
            x = np.asarray(inputs["x"], dtype=np.float32)
            m["x"] = np.ascontiguousarray(x[b])
            m["xT"] = np.ascontiguousarray(x[b].T)
        in_maps.append(m)
    res = run_bass_kernel_spmd(nc, in_maps, core_ids=list(range(8)))
    out = np.stack([np.asarray(r["out"], dtype=np.float32) for r in res.results], axis=0)
    return out
```

```python
import contextlib
import os
import numpy as np
import concourse.bass as bass
import concourse.mybir as mybir
from concourse.bass_utils import run_bass_kernel_spmd

F32 = mybir.dt.float32
BF16 = mybir.dt.bfloat16
AF = mybir.ActivationFunctionType
ALU = mybir.AluOpType
AX = mybir.AxisListType

PE, DVE, ACT, POOL, SP = "tensor", "vector", "scalar", "gpsimd", "sync"
ENGS = (PE, DVE, ACT, POOL, SP)
NDMASEM = 8
ATTACH = os.environ.get('KATTACH', '1') == '1'
WAR_SKIP = {'0': (), 'DA': (DVE, ACT), 'DAP': (DVE, ACT, POOL)}[os.environ.get('KWAR', '0')]

L = 2048
D = 1024
NT = 16
KC = 8
NH = 8
PROJ = 8208
NE = 16
FF = 256
ALPHA = 2.0 ** 0.25
LN_EPS = 1e-5
NORM_EPS = 1e-6
C_QKV, C_Z, C_A, C_UV, C_GDN, C_GSGU = 0, 3072, 4096, 4112, 6160, 7184


class Buf:
    __slots__ = ("name", "writers", "readers", "excl")

    def __init__(self, name, excl=False):
        self.name = name
        self.excl = excl
        self.writers = {}
        self.readers = {}


class Op:
    __slots__ = ("eng", "fn", "raw", "war", "is_dma", "signal", "count", "dsem", "dcount", "prev_dma", "key")

    def __init__(self, eng, fn, is_dma):
        self.eng = eng
        self.fn = fn
        self.raw = set()
        self.war = set()
        self.is_dma = is_dma
        self.signal = False
        self.count = None
        self.dsem = None
        self.dcount = None
        self.prev_dma = None
        self.key = eng


class Builder:
    def __init__(self, nc):
        self.nc = nc
        self.ops = []
        self.dma_rr = {e: 0 for e in ENGS}
        self.dma_last = {}

    def add(self, eng, fn, reads=(), writes=(), dma=False, accumulate=False):
        op = Op(eng, fn, dma)
        if dma:
            slot = self.dma_rr[eng] % NDMASEM
            self.dma_rr[eng] += 1
            op.dsem = (eng, slot)
            op.key = (eng, slot)
            prev = self.dma_last.get((eng, slot))
            op.prev_dma = prev
            op.dcount = (prev.dcount if prev is not None else 0) + 16
            self.dma_last[(eng, slot)] = op
        for b in reads:
            op.raw.update(b.writers.values())
            if b.excl:
                for kk, v in b.readers.items():
                    if v.eng != eng:
                        op.raw.add(v)
        for b in writes:
            if accumulate:
                op.war.update(v for v in b.writers.values() if not v.is_dma)
            else:
                op.war.update(b.writers.values())
            op.war.update(b.readers.values())
        for b in reads:
            b.readers[op.key] = op
        for b in writes:
            if accumulate:
                b.writers = {kk: v for kk, v in b.writers.items() if v.is_dma}
                b.writers[op.key] = op
            else:
                b.writers = {op.key: op}
            b.readers = {}
        self.ops.append(op)
        return op

    def emit(self):
        nc = self.nc
        ops = self.ops
        for op in ops:
            deps = set()
            for d in op.raw:
                if d is op:
                    continue
                if (not d.is_dma) and (not op.is_dma) and d.eng == op.eng and op.eng == PE:
                    continue
                deps.add(d)
            for d in op.war:
                if d is op or d in deps:
                    continue
                if (not d.is_dma) and (not op.is_dma) and d.eng == op.eng and (op.eng == PE or (op.eng in WAR_SKIP)):
                    continue
                deps.add(d)
            op.raw = deps
            for d in deps:
                if not d.is_dma:
                    d.signal = True
        self.maxwait = {}
        counts = {e: 0 for e in ENGS}
        for op in ops:
            if (not op.is_dma) and op.signal:
                counts[op.eng] += 1
                op.count = counts[op.eng]
        if os.environ.get('KVERBOSE'):
            print('signal counts', counts, 'nops', len(ops), 'dma', {k_: v.dcount for k_, v in self.dma_last.items()})
        kn_all = {e: {} for e in ENGS}
        todos = {}
        snaps = {}
        for op in ops:
            kn = kn_all[op.eng]
            need, src = {}, {}
            for d in op.raw:
                if d.is_dma:
                    key, val = ("d",) + d.dsem, d.dcount
                else:
                    key, val = ("e", d.eng), d.count
                if val > need.get(key, 0):
                    need[key], src[key] = val, d
            if op.is_dma and op.prev_dma is not None:
                key = ("d",) + op.dsem
                if op.prev_dma.dcount > need.get(key, 0):
                    need[key], src[key] = op.prev_dma.dcount, op.prev_dma
            todo = []
            for key, val in need.items():
                if kn.get(key, 0) >= val:
                    continue
                todo.append((key, val))
                kn[key] = val
                for k2, v2 in snaps[id(src[key])].items():
                    if kn.get(k2, 0) < v2:
                        kn[k2] = v2
            todos[id(op)] = todo
            if op.is_dma or op.signal:
                snaps[id(op)] = dict(kn)
        with contextlib.ExitStack() as st:
            esem = {e: st.enter_context(nc.semaphore(f"s_{e}")) for e in (PE, DVE, ACT, POOL)}
            dsem = {}
            for e in (SP, POOL):
                for s in range(NDMASEM):
                    dsem[(e, s)] = st.enter_context(nc.semaphore(f"d_{e}_{s}"))
            block = st.enter_context(nc.Block())

            def run_engine(eng_name, eng):
                known = {}
                for op in ops:
                    if op.eng != eng_name:
                        continue
                    todo = []
                    for key, val in todos[id(op)]:
                        sem = dsem[key[1:]] if key[0] == "d" else esem[key[1]]
                        self.maxwait[key] = max(self.maxwait.get(key, 0), val)
                        todo.append((sem, val))
                    attach = None
                    if ATTACH and todo and not op.is_dma:
                        attach = todo.pop()
                    for sem, val in todo:
                        eng.wait_ge(sem, val)
                    ins = op.fn(eng)
                    if attach is not None:
                        ins._wait_ge(attach[0], attach[1])
                    if op.is_dma:
                        ins.then_inc(dsem[op.dsem], 16)
                    elif op.signal:
                        ins.then_inc(esem[op.eng], 1)
                if eng_name == SP:
                    for key, last in self.dma_last.items():
                        eng.wait_ge(dsem[key], last.dcount)

            @block.tensor
            def _(eng):
                run_engine(PE, eng)

            @block.vector
            def _(eng):
                run_engine(DVE, eng)

            @block.scalar
            def _(eng):
                run_engine(ACT, eng)

            @block.gpsimd
            def _(eng):
                run_engine(POOL, eng)

            @block.sync
            def _(eng):
                run_engine(SP, eng)


class Reg:
    def __init__(self, arena, off, n, buf):
        self.arena, self.off, self.n, self.buf = arena, off, n, buf

    def f(self, a=0, b=None):
        b = self.n if b is None else b
        return self.arena[:, self.off + a:self.off + b]

    def h(self, a=0, b=None):
        v = self.arena[:, self.off:self.off + self.n].bitcast(BF16)
        b = 2 * self.n if b is None else b
        return v[:, a:b]


class _GatedBuilder(Builder):
    def __init__(self, nc, kb):
        super().__init__(nc)
        self.kb = kb

    def add(self, *a, **kw):
        if not self.kb.enabled:
            return None
        return super().add(*a, **kw)


class KB:
    def __init__(self, nc, arena, arena_cols, psum):
        self.nc = nc
        self.B = _GatedBuilder(nc, self)
        self.arena = arena
        self.cols = arena_cols
        self.top = 0
        self.dead = []
        self.live = []
        self.registry = {}
        self.psum = psum
        self.pbufs = [Buf(f"ps{i}", excl=True) for i in range(8)]
        self.prr = 0
        self.enabled = True
        self.phase_on = True
        self.sub_on = True
        self.stop = float(os.environ.get('KSTOP', '99'))

    def alloc(self, name, n, at=None):
        n = (n + 7) // 8 * 8
        off = self.top if at is None else at
        assert off + n <= self.cols, f"arena overflow at {name}: {off + n} > {self.cols}"
        for r in self.live:
            assert not (r.off < off + n and off < r.off + r.n), f"{name} overlaps live {r.buf.name}"
        buf = Buf(name)
        for r in self.dead:
            if r.off < off + n and off < r.off + r.n:
                ob = r.buf
                for kk, v in ob.readers.items():
                    buf.readers[("x", id(ob), kk)] = v
                for kk, v in ob.writers.items():
                    buf.readers[("w", id(ob), kk)] = v
        reg = Reg(self.arena, off, n, buf)
        self.registry[name] = reg
        self.live.append(reg)
        self.top = off + n
        return reg

    def kill(self, regs):
        for r in regs:
            self.live.remove(r)
            self.dead.append(r)

    def sub(self, n):
        lim = float(os.environ.get('KSUB', '99'))
        if getattr(self, 'cur_hh', 0) == 1 and os.environ.get('KSUB1'):
            lim = float(os.environ['KSUB1'])
        self.sub_on = n <= lim
        self.enabled = self.sub_on and self.phase_on

    def phase(self, n):
        self.phase_on = n <= self.stop
        self.sub_on = True
        self.enabled = self.phase_on

    def bank(self, i=None):
        if i is None:
            i = self.prr % 8
            self.prr += 1
        return self.psum[:, i * 512:(i + 1) * 512], self.pbufs[i]

    def mm(self, out, lhsT, rhs, start, stop, r, w):
        self.B.add(PE, lambda e: e.matmul(out, lhsT=lhsT, rhs=rhs, start=start, stop=stop), r, w)

    def tr(self, out, in_, ident, r, w):
        self.B.add(PE, lambda e: e.matmul(out, lhsT=in_, rhs=ident, start=True, stop=True), r, w)

    def act(self, out, in_, func, r, w, bias=None, scale=None, accum=None):
        kw = {}
        if bias is not None:
            kw["bias"] = bias
        if scale is not None:
            kw["scale"] = scale
        if accum is not None:
            kw["accum_out"] = accum
        self.B.add(ACT, lambda e: e.activation(out=out, in_=in_, func=func, **kw), r, w)

    def tt(self, out, a, b, op, r, w, eng=DVE):
        self.B.add(eng, lambda e: e.tensor_tensor(out=out, in0=a, in1=b, op=op), r, w)

    def ts(self, out, a, s1, s2, op0, op1, r, w, eng=DVE):
        if op1 is None:
            self.B.add(eng, lambda e: e.tensor_scalar(out=out, in0=a, scalar1=s1, scalar2=None, op0=op0), r, w)
        else:
            self.B.add(eng, lambda e: e.tensor_scalar(out=out, in0=a, scalar1=s1, scalar2=s2, op0=op0, op1=op1), r, w)

    def stt(self, out, a, s, b, op0, op1, r, w, eng=DVE):
        self.B.add(eng, lambda e: e.scalar_tensor_tensor(out=out, in0=a, scalar=s, in1=b, op0=op0, op1=op1), r, w)

    def rsqrt(self, out, in_, eps, r, w):
        self.ts(out, in_, eps, None, ALU.add, None, r, w)
        self.act(out, out, AF.Ln, w, w)
        self.act(out, out, AF.Exp, w, w, scale=-0.5)

    def cp(self, out, in_, r, w, eng=DVE):
        self.B.add(eng, lambda e: e.tensor_copy(out=out, in_=in_), r, w)

    def memset(self, out, val, w, eng=POOL):
        self.B.add(eng, lambda e: e.memset(out, val), (), w)

    def dma(self, out, in_, r, w, q=SP, acc=False):
        self.B.add(q, lambda e: e.dma_start(out=out, in_=in_), r, w, dma=True, accumulate=acc)

    def generic(self, eng, fn, r, w):
        self.B.add(eng, fn, r, w)


def _consts():
    i = np.arange(128)
    t = i[:, None]
    s = i[None, :]
    tabs = {}
    tabs["ident"] = (t == s)
    tabs["U"] = (t <= s)
    tabs["SL"] = (t > s)
    tabs["UI"] = (t <= s)
    tabs["ones"] = np.ones((128, 128), bool)
    tabs["D16"] = (t // 16 == s // 16) & (t > s)
    tabs["D16T"] = tabs["D16"].T
    for b in (16, 32, 64):
        em = (t // (2 * b) == s // (2 * b)) & ((t % (2 * b)) >= b) & ((s % (2 * b)) < b)
        tabs[f"E{b}"] = em
        tabs[f"F{b}"] = em.T
    names = list(tabs)
    arr = np.concatenate([tabs[n].astype(np.float32) for n in names], axis=1)
    return names, np.ascontiguousarray(arr)


CNAMES, CARR = _consts()


def build_program(debug=None):
    nc = bass.Bass("TRN2", target_bir_lowering=False)

    def din(name, shape):
        return nc.dram_tensor(name, list(shape), F32, kind="ExternalInput").ap()

    xT_d = din("xT", (D, L))
    x_d = din("x", (L, D))
    w_in = din("w_in", (D, PROJ))
    convT = din("convT", (128, 24 * 4))
    a_log = din("a_log", (1, NH))
    dt_bias = din("dt_bias", (1, NH))
    dn_norm_g = din("dn_norm_g", (1, 128))
    w_dn_out = din("w_dn_out", (D, D))
    sgu_ln_g = din("sgu_ln_g", (1, D))
    sgu_ln_b = din("sgu_ln_b", (1, D))
    spwT = din("spwT", (8, 128, 128))
    spb = din("spb", (1, 8 * 128))
    w_sgu_out = din("w_sgu_out", (D, D))
    w_out = din("w_out", (D, D))
    ln1_g = din("ln1_g", (1, D))
    ln1_b = din("ln1_b", (1, D))
    w_rt = din("w_rt", (D, 20))
    b_rt = din("b_rt", (1, 20))
    ew_gate = din("ew_gate", (NE, D, FF))
    ew_up = din("ew_up", (NE, D, FF))
    ew_down = din("ew_down", (NE, FF, D))
    ln2_g = din("ln2_g", (1, D))
    ln2_b = din("ln2_b", (1, D))
    ctab = din("ctab", (128, CARR.shape[1]))
    out_d = nc.dram_tensor("out", [L, D], F32, kind="ExternalOutput").ap()
    h1_d = nc.dram_tensor("h1s", [L, D], F32, kind="Internal").ap()
    dbg = {}
    if debug:
        for name, shape in debug.items():
            dbg[name] = nc.dram_tensor("dbg_" + name, list(shape), F32, kind="ExternalOutput").ap()

    AW = 53200
    with contextlib.ExitStack() as st:
        arena = st.enter_context(nc.sbuf_tensor("arena", [128, AW], F32))
        psum = st.enter_context(nc.psum_tensor("psum", [128, 4096], F32))
        k = KB(nc, arena, AW, psum)
        h1bufs = [Buf(f"h1d{i}") for i in range(NT)]

        def bcast_load(reg, src_row, n, a=0):
            k.dma(reg.f(a, a + n).unsqueeze(1), src_row.partition_broadcast(128), (), [reg.buf], acc=True)

        if os.environ.get('KZERO', '0') == '1':
            zb = Buf('zero')
            for z0 in range(0, AW, 6400):
                k.memset(arena[:, z0:z0 + 6400], 0.0, [zb], eng=DVE if (z0 // 6400) % 2 else POOL)
            k.dead.append(Reg(arena, 0, AW, zb))
        nct = len(CNAMES)
        cf = k.alloc("cf", nct * 128)
        k.dma(cf.f(), ctab, (), [cf.buf])
        cb = k.alloc("cb", nct * 64)
        k.dma(cb.h(), ctab, (), [cb.buf], q=POOL)

        def CF(name):
            j = CNAMES.index(name)
            return cf.f(j * 128, (j + 1) * 128)

        def CBh(name):
            j = CNAMES.index(name)
            return cb.h(j * 128, (j + 1) * 128)

        xT = k.alloc("xT", KC * L // 2)
        xTv = xT.h().rearrange("p (c t) -> p c t", t=L)
        for c in range(KC):
            k.dma(xTv[:, c, :], xT_d[c * 128:(c + 1) * 128, :], (), [xT.buf], q=POOL, acc=True)
        oT = k.alloc("oT", NH * L // 2)
        oTv = oT.h().rearrange("p (h t) -> p h t", t=L)

        mark_dn = k.top
        k.phase(1)
        small = k.alloc("small", 11 * 128)
        sm = lambda j: small.f(j * 128, (j + 1) * 128)
        G_, BETA, EG, EGL, EGLMG, BEG, GG, TMP, TMP2, AB0, AB1 = range(11)
        prm = k.alloc("prm", 16 + 128)
        bcast_load(prm, a_log, NH, 0)
        bcast_load(prm, dt_bias, NH, 8)
        bcast_load(prm, dn_norm_g, 128, 16)
        wab = k.alloc("wab", KC * 16 // 2)
        wabv = wab.h().rearrange("p (c n) -> p c n", n=16)
        k.dma(wabv, w_in[:, C_A:C_A + 16].rearrange("(c p) n -> p c n", p=128), (), [wab.buf], q=POOL)
        abv = small.f(AB0 * 128, AB0 * 128 + 256).rearrange("p (t n) -> p t n", n=16)
        for i in range(NT):
            pb, pbuf = k.bank()
            for c in range(KC):
                k.mm(pb[:, 0:16], xTv[:, c, i * 128:(i + 1) * 128], wabv[:, c, :], c == 0, c == KC - 1,
                     [xT.buf, wab.buf], [pbuf])
            k.act(abv[:, i, :], pb[:, 0:16], AF.Copy, [pbuf], [small.buf])
        a_v = abv[:, :, 0:8]
        b_v = abv[:, :, 8:16]
        v3 = lambda j: sm(j).rearrange("p (t n) -> p t n", n=8)
        sb = [small.buf]
        k.tt(v3(TMP), a_v, prm.f(8, 16).unsqueeze(1).to_broadcast([128, NT, 8]), ALU.add, sb + [prm.buf], sb)
        k.act(sm(TMP), sm(TMP), AF.Exp, sb, sb)
        k.ts(sm(TMP), sm(TMP), 1.0, None, ALU.add, None, sb, sb)
        k.act(sm(TMP), sm(TMP), AF.Ln, sb, sb)
        k.act(prm.f(0, 8), prm.f(0, 8), AF.Exp, [prm.buf], [prm.buf])
        k.stt(v3(GG), v3(TMP), -1.0, prm.f(0, 8).unsqueeze(1).to_broadcast([128, NT, 8]), ALU.mult, ALU.mult,
              sb + [prm.buf], sb)
        k.act(v3(BETA), b_v, AF.Exp, sb, sb, scale=-1.0)
        k.ts(sm(BETA), sm(BETA), 1.0, None, ALU.add, None, sb, sb)
        k.generic(DVE, lambda e: e.reciprocal(out=sm(BETA), in_=sm(BETA)), sb, sb)
        pb, pbuf = k.bank()
        k.mm(pb[:, 0:128], CF("U"), sm(GG), True, True, [cf.buf] + sb, [pbuf])
        k.mm(pb[:, 128:256], CF("ones"), sm(GG), True, True, [cf.buf] + sb, [pbuf])
        k.act(sm(G_), pb[:, 0:128], AF.Copy, [pbuf], sb)
        k.act(sm(EG), pb[:, 0:128], AF.Exp, [pbuf], sb)
        k.act(sm(EGL), pb[:, 128:256], AF.Exp, [pbuf], sb)
        k.tt(sm(TMP2), pb[:, 128:256], sm(G_), ALU.subtract, [pbuf] + sb, sb)
        k.act(sm(EGLMG), sm(TMP2), AF.Exp, sb, sb)
        k.tt(sm(BEG), sm(BETA), sm(EG), ALU.mult, sb, sb)
        k.ts(prm.f(16, 144), prm.f(16, 144), float(np.sqrt(128.0)), None, ALU.mult, None, [prm.buf], [prm.buf])

        if os.environ.get('KNAN'):
            nn = k.alloc('nantest', 8)
            k.memset(nn.f(), -1.0, [nn.buf])
            k.act(nn.f(), nn.f(), AF.Ln, [nn.buf], [nn.buf])
            k.kill([nn])
            k.top = nn.off
        k.phase(2)
        cw = k.alloc("cw", 96)
        k.dma(cw.f(), convT, (), [cw.buf])
        cinL = [k.alloc(f"cin{j}", (4 + L + 12) // 2) for j in range(2)]
        dgL = [k.alloc(f"dg{j}", 4 * 64) for j in range(2)]
        caccL = [k.alloc(f"cacc{j}", L) for j in range(2)]
        csqL = [k.alloc(f"csq{j}", L // 2) for j in range(2)]
        _crn = k.alloc("crn", 512)
        crnL = [_crn, _crn]
        for cin_ in cinL:
            k.memset(cin_.h(0, 4), 0.0, [cin_.buf])
        _wp = k.alloc("wp", 4 * KC * 256 // 2)
        wp = [_wp, _wp]
        qkv = k.alloc("qkvT", 6 * L // 2)
        qkvv = qkv.h().rearrange("p (c t) -> p c t", t=L)
        szr = k.alloc("sz", NT * 256 // 2)
        szv = szr.h().rearrange("p (t n) -> p t n", n=256)
        S32 = k.alloc("S32", 2 * 128)
        Sbf = k.alloc("Sbf", 2 * 64)
        NU = 4
        UNIT_REGS = (("tA", 64), ("tB", 64), ("tA1", 64), ("tB1", 64), ("tA2", 128), ("tE0", 64), ("tE1", 64),
                     ("tE2", 64), ("tF0", 64), ("tF1", 64), ("tR", 64), ("tT", 64), ("tZ", 128), ("tD", 256),
                     ("tX", 256), ("tGUh", 128), ("tkbd", 64), ("tvb", 64))
        HAND_REGS = (("tqk", 64), ("tkdec", 64), ("tu", 128), ("tWT", 64))
        UT = [dict() for _ in range(NU)]
        for nm, n in UNIT_REGS:
            for u in range(NU):
                UT[u][nm] = k.alloc(f"{nm}_{u}", n)

        HT = [[{nm: k.alloc(f"{nm}_h{par}_{u}", n) for nm, n in HAND_REGS} for u in range(NU)] for par in range(2)]

        def gview(nm):
            r0 = UT[0][nm]
            return arena[:, r0.off:r0.off + NU * r0.n].bitcast(BF16).rearrange("p (u c) -> p u c", c=128)

        def gbufs(nm):
            return [UT[u][nm].buf for u in range(NU)]
        PT = [{nm: k.alloc(f"{nm}_p{u}", n) for nm, n in (("tvn", 64), ("to1", 128), ("to", 128), ("tof", 64), ("tss", 8))}
              for u in range(2)]

        def load_pair_weights(p, wr):
            wv = wr.h().rearrange("p (j c n) -> p j c n", j=4, n=256)
            for j, c0 in enumerate((C_QKV + p * 256, C_QKV + 1024 + p * 256, C_QKV + 2048 + p * 256, C_Z + p * 256)):
                k.dma(wv[:, j], w_in[:, c0:c0 + 256].rearrange("(c p) n -> p c n", p=128), (), [wr.buf], q=POOL, acc=True)
            return wv

        for p in range(int(os.environ.get('KPAIRS', '4'))):
            k.phase(2.1)
            wr = wp[0]
            wv = wv_next if p > 0 else load_pair_weights(0, wr)
            identb_ = CBh("ident")
            for ct in range(6):
                j, hh = ct // 2, ct % 2
                gct = j * 8 + 2 * p + hh
                cin, cacc, csq = cinL[ct % 2], caccL[ct % 2], csqL[ct % 2]
                dg = dgL[ct % 2]
                for tap in range(4):
                    k.ts(dg.h(tap * 128, (tap + 1) * 128), identb_, cw.f(gct * 4 + tap, gct * 4 + tap + 1), None, ALU.mult, None,
                         [cb.buf, cw.buf], [dg.buf])
                for tb in range(4):
                    pb, pbuf = k.bank()
                    for c in range(KC):
                        k.mm(pb, wv[:, j, c, hh * 128:(hh + 1) * 128], xTv[:, c, tb * 512:(tb + 1) * 512],
                             c == 0, c == KC - 1, [wr.buf, xT.buf], [pbuf])
                    k.act(cin.h(4 + tb * 512, 4 + (tb + 1) * 512), pb, AF.Copy, [pbuf], [cin.buf])
                for tb in range(4):
                    pb, pbuf = k.bank()
                    for tap in range(4):
                        k.mm(pb, dg.h(tap * 128, (tap + 1) * 128), cin.h(1 + tb * 512 + tap, 1 + tb * 512 + tap + 512),
                             tap == 0, tap == 3, [dg.buf, cin.buf], [pbuf])
                    if j == 2:
                        k.act(qkvv[:, ct, tb * 512:(tb + 1) * 512], pb, AF.Silu, [pbuf], [qkv.buf])
                    else:
                        k.act(cacc.f(tb * 512, (tb + 1) * 512), pb, AF.Silu, [pbuf], [cacc.buf])
                if j != 2:
                    k.act(csq.h(), cacc.f(), AF.Square, [cacc.buf], [csq.buf])
                    for tb in range(4):
                        pb, pbuf = k.bank()
                        crn = crnL[tb % 2]
                        k.mm(pb, CBh("ones"), csq.h(tb * 512, (tb + 1) * 512), True, True, [cb.buf, csq.buf], [pbuf])
                        k.rsqrt(crn.f(), pb, NORM_EPS, [pbuf], [crn.buf])
                        k.stt(qkvv[:, ct, tb * 512:(tb + 1) * 512], cacc.f(tb * 512, (tb + 1) * 512),
                              (128.0 ** -0.5) if j == 0 else 1.0, crn.f(), ALU.mult, ALU.mult,
                              [cacc.buf, crn.buf], [qkv.buf])
            k.phase(2.2)
            for i in range(NT):
                pb, pbuf = k.bank()
                for c in range(KC):
                    k.mm(pb[:, 0:256], xTv[:, c, i * 128:(i + 1) * 128], wv[:, 3, c, :], c == 0, c == KC - 1,
                         [xT.buf, wr.buf], [pbuf])
                k.act(szv[:, i, :], pb[:, 0:256], AF.Silu, [pbuf], [szr.buf])
            if p < 3:
                wv_next = load_pair_weights(p + 1, wr)
            k.memset(S32.f(), 0.0, [S32.buf])
            k.memset(Sbf.h(), 0.0, [Sbf.buf])
            identb = CBh("ident")

            def pre(i, hh, T):
                ts_ = slice(i * 128, (i + 1) * 128)
                h = 2 * p + hh
                col = i * 8 + h
                sc = lambda j: sm(j)[:, col:col + 1]
                qT = qkvv[:, 0 + hh, ts_]
                kT = qkvv[:, 2 + hh, ts_]
                vT = qkvv[:, 4 + hh, ts_]
                tA, tB, tA1, tB1, tA2, tR, tT, tZ, tD, tX = (T[n] for n in ("tA", "tB", "tA1", "tB1", "tA2", "tR", "tT", "tZ", "tD", "tX"))
                tGUh, tqk, tkbd, tkdec, tvb, tu, tWT = (T[n] for n in ("tGUh", "tqk", "tkbd", "tkdec", "tvb", "tu", "tWT"))
                tE = [T["tE0"], T["tE1"], T["tE2"]]
                tF = [T["tF0"], T["tF1"]]
                pb, pbuf = k.bank()
                k.tr(pb[:, 0:128], kT, identb, [qkv.buf, cb.buf], [pbuf])
                k.tr(pb[:, 128:256], vT, identb, [qkv.buf, cb.buf], [pbuf])
                k.act(tkbd.h(), pb[:, 0:128], AF.Identity, [pbuf, small.buf], [tkbd.buf], scale=sc(BEG))
                k.act(tvb.h(), pb[:, 128:256], AF.Identity, [pbuf, small.buf], [tvb.buf], scale=sc(BETA))
                k.ts(tkdec.h(), pb[:, 0:128], sc(EGLMG), None, ALU.mult, None, [pbuf, small.buf], [tkdec.buf])
                k.act(tGUh.h(0, 128), CF("U"), AF.Identity, [cf.buf, small.buf], [tGUh.buf], scale=sc(GG))
                k.stt(tGUh.h(128, 256), CF("U"), sc(GG), tGUh.h(0, 128), ALU.mult, ALU.subtract,
                      [cf.buf, small.buf, tGUh.buf], [tGUh.buf])
                yield
                pb, pbuf = k.bank()
                k.mm(pb[:, 0:128], kT, kT, True, True, [qkv.buf], [pbuf])
                k.mm(pb[:, 128:256], kT, qT, True, True, [qkv.buf], [pbuf])
                k.mm(pb[:, 256:384], tGUh.h(0, 128), CBh("SL"), True, False, [tGUh.buf, cb.buf], [pbuf])
                k.mm(pb[:, 256:384], tGUh.h(128, 256), CBh("SL"), False, True, [tGUh.buf, cb.buf], [pbuf])
                k.mm(pb[:, 384:512], CBh("SL"), tGUh.h(0, 128), True, False, [tGUh.buf, cb.buf], [pbuf])
                k.mm(pb[:, 384:512], CBh("SL"), tGUh.h(128, 256), False, True, [tGUh.buf, cb.buf], [pbuf])
                k.act(tD.f(), pb[:, 256:512], AF.Exp, [pbuf], [tD.buf])
                k.tt(tX.f(), tD.f(), pb[:, 0:256], ALU.mult, [tD.buf, pbuf], [tX.buf])
                k.stt(tA.h(), tX.f(0, 128), sc(BETA), CF("SL"), ALU.mult, ALU.mult,
                      [tX.buf, small.buf, cf.buf], [tA.buf])
                k.tt(tqk.h(), tX.f(128, 256), CF("UI"), ALU.mult, [tX.buf, cf.buf], [tqk.buf])
                yield
                pb, pbuf = k.bank()
                k.tr(pb[:, 0:128], tA.h(), identb, [tA.buf, cb.buf], [pbuf])
                k.act(tB.h(), pb[:, 0:128], AF.Copy, [pbuf], [tB.buf])
                yield

            def group_masks():
                def bc(name):
                    return CBh(name).unsqueeze(1).to_broadcast([128, NU, 128])
                k.tt(gview("tA1"), gview("tA"), bc("D16"), ALU.mult, gbufs("tA") + [cb.buf], gbufs("tA1"))
                k.tt(gview("tB1"), gview("tB"), bc("D16T"), ALU.mult, gbufs("tB") + [cb.buf], gbufs("tB1"))
                k.tt(gview("tR"), bc("ident"), gview("tB1"), ALU.subtract, gbufs("tB1") + [cb.buf], gbufs("tR"))
                k.tt(gview("tT"), bc("ident"), gview("tA1"), ALU.subtract, gbufs("tA1") + [cb.buf], gbufs("tT"))
                for li, b in enumerate((16, 32, 64)):
                    k.tt(gview(f"tE{li}"), gview("tA"), bc(f"E{b}"), ALU.mult, gbufs("tA") + [cb.buf], gbufs(f"tE{li}"))
                    if b < 64:
                        k.tt(gview(f"tF{li}"), gview("tB"), bc(f"F{b}"), ALU.mult, gbufs("tB") + [cb.buf], gbufs(f"tF{li}"))

            def pre_b(i, hh, T):
                tA, tB, tA1, tB1, tA2, tR, tT, tZ = (T[n] for n in ("tA", "tB", "tA1", "tB1", "tA2", "tR", "tT", "tZ"))
                tkbd, tvb, tu, tWT = (T[n] for n in ("tkbd", "tvb", "tu", "tWT"))
                tE = [T["tE0"], T["tE1"], T["tE2"]]
                tF = [T["tF0"], T["tF1"]]
                curA, curB, curAb, curBb = tA1.h(), tB1.h(), tA1.buf, tB1.buf
                for lev in range(3):
                    pb, pbuf = k.bank()
                    k.mm(pb[:, 0:128], curB, curA, True, True, [curAb, curBb], [pbuf])
                    k.mm(pb[:, 128:256], curA, curB, True, True, [curAb, curBb], [pbuf])
                    k.act(tA2.h(), pb[:, 0:256], AF.Copy, [pbuf], [tA2.buf])
                    yield
                    nA, nB = tA2.h(0, 128), tA2.h(128, 256)
                    pb2, pbuf2 = k.bank()
                    k.mm(pb2[:, 0:128], nA, tR.h(), True, True, [tA2.buf, tR.buf], [pbuf2])
                    k.mm(pb2[:, 128:256], nB, tT.h(), True, True, [tA2.buf, tT.buf], [pbuf2])
                    k.tt(tR.h(), tR.h(), pb2[:, 0:128], ALU.add, [tR.buf, pbuf2], [tR.buf])
                    k.tt(tT.h(), tT.h(), pb2[:, 128:256], ALU.add, [tT.buf, pbuf2], [tT.buf])
                    curA, curB, curAb, curBb = tA2.h(0, 128), tA2.h(128, 256), tA2.buf, tA2.buf
                    yield
                for li, b in enumerate((16, 32, 64)):
                    pb, pbuf = k.bank()
                    k.mm(pb[:, 0:128], tE[li].h(), tR.h(), True, True, [tE[li].buf, tR.buf], [pbuf])
                    if b < 64:
                        k.mm(pb[:, 128:256], tF[li].h(), tT.h(), True, True, [tF[li].buf, tT.buf], [pbuf])
                        k.act(tZ.h(), pb[:, 0:256], AF.Copy, [pbuf], [tZ.buf])
                    else:
                        k.act(tZ.h(0, 128), pb[:, 0:128], AF.Copy, [pbuf], [tZ.buf])
                    yield
                    pb2, pbuf2 = k.bank()
                    k.mm(pb2[:, 0:128], tT.h(), tZ.h(0, 128), True, True, [tT.buf, tZ.buf], [pbuf2])
                    if b < 64:
                        k.mm(pb2[:, 128:256], tR.h(), tZ.h(128, 256), True, True, [tR.buf, tZ.buf], [pbuf2])
                    k.tt(tR.h(), tR.h(), pb2[:, 0:128], ALU.subtract, [tR.buf, pbuf2], [tR.buf])
                    if b < 64:
                        k.tt(tT.h(), tT.h(), pb2[:, 128:256], ALU.subtract, [tT.buf, pbuf2], [tT.buf])
                    yield
                pb, pbuf = k.bank()
                k.mm(pb[:, 0:128], tR.h(), tvb.h(), True, True, [tR.buf, tvb.buf], [pbuf])
                k.mm(pb[:, 128:256], tkbd.h(), tR.h(), True, True, [tR.buf, tkbd.buf], [pbuf])
                k.act(tu.f(), pb[:, 0:128], AF.Copy, [pbuf], [tu.buf])
                k.act(tWT.h(), pb[:, 128:256], AF.Copy, [pbuf], [tWT.buf])
                yield

            def post(i, hh, T, P):
                ts_ = slice(i * 128, (i + 1) * 128)
                h = 2 * p + hh
                col = i * 8 + h
                sc = lambda j: sm(j)[:, col:col + 1]
                qT = qkvv[:, 0 + hh, ts_]
                tqk, tkdec, tu, tWT = T["tqk"], T["tkdec"], T["tu"], T["tWT"]
                tvn, to1, to, tof, tss = P["tvn"], P["to1"], P["to"], P["tof"], P["tss"]
                Sh = Sbf.h(hh * 128, (hh + 1) * 128)
                Sf = S32.f(hh * 128, (hh + 1) * 128)
                pb, pbuf = k.bank()
                k.mm(pb[:, 0:128], tWT.h(), Sh, True, True, [tWT.buf, Sbf.buf], [pbuf])
                k.mm(pb[:, 128:256], qT, Sh, True, True, [qkv.buf, Sbf.buf], [pbuf])
                k.tt(tvn.h(), tu.f(), pb[:, 0:128], ALU.subtract, [tu.buf, pbuf], [tvn.buf])
                k.act(to1.f(), pb[:, 128:256], AF.Identity, [pbuf, small.buf], [to1.buf], scale=sc(EG))
                yield
                pb2, pbuf2 = k.bank()
                k.mm(pb2[:, 0:128], tqk.h(), tvn.h(), True, True, [tqk.buf, tvn.buf], [pbuf2])
                k.mm(pb2[:, 128:256], tkdec.h(), tvn.h(), True, True, [tkdec.buf, tvn.buf], [pbuf2])
                k.stt(Sf, Sf, sc(EGL), pb2[:, 128:256], ALU.mult, ALU.add, [S32.buf, small.buf, pbuf2], [S32.buf])
                k.act(Sh, Sf, AF.Copy, [S32.buf], [Sbf.buf])
                k.tt(to.f(), to1.f(), pb2[:, 0:128], ALU.add, [to1.buf, pbuf2], [to.buf])
                yield
                k.act(to1.f(), to.f(), AF.Square, [to.buf, tss.buf], [to1.buf, tss.buf], accum=tss.f(0, 1))
                k.rsqrt(tss.f(1, 2), tss.f(0, 1), 128.0 * NORM_EPS, [tss.buf], [tss.buf])
                k.stt(to.f(), to.f(), tss.f(1, 2), prm.f(16, 144), ALU.mult, ALU.mult,
                      [to.buf, tss.buf, prm.buf], [to.buf])
                k.tt(tof.h(), to.f(), szv[:, i, hh * 128:(hh + 1) * 128], ALU.mult, [to.buf, szr.buf], [tof.buf])
                yield
                pb, pbuf = k.bank()
                k.tr(pb[:, 0:128], tof.h(), identb, [tof.buf, cb.buf], [pbuf])
                k.act(oTv[:, h, ts_], pb[:, 0:128], AF.Copy, [pbuf], [oT.buf])
                yield

            def lockstep(gens):
                gens = list(gens)
                while gens:
                    nxt = []
                    for g_ in gens:
                        try:
                            next(g_)
                            nxt.append(g_)
                        except StopIteration:
                            pass
                    gens = nxt

            TG = NU // 2
            groups = list(range(0, NT, TG))

            def Tm(gi, u):
                d = dict(UT[u])
                d.update(HT[gi % 2][u])
                return d

            def step(gens):
                alive = []
                for g_ in gens:
                    try:
                        next(g_)
                        alive.append(g_)
                    except StopIteration:
                        pass
                return alive

            def pre_group(gi):
                g0 = groups[gi]
                units = [(i, hh) for i in range(g0, g0 + TG) for hh in range(2)]
                gens = [pre(i, hh, Tm(gi, u)) for u, (i, hh) in enumerate(units)]
                while gens:
                    gens = step(gens)
                    yield
                group_masks()
                yield
                gens = [pre_b(i, hh, Tm(gi, u)) for u, (i, hh) in enumerate(units)]
                while gens:
                    gens = step(gens)
                    yield

            def post_group(gi):
                g0 = groups[gi]
                for i in range(g0, g0 + TG):
                    gens = [post(i, hh, Tm(gi, (i - g0) * 2 + hh), PT[hh]) for hh in range(2)]
                    while gens:
                        gens = step(gens)
                        yield

            def run2(a_, b_):
                threads = [t for t in (a_, b_) if t is not None]
                while threads:
                    threads = step(threads)

            run2(pre_group(0), None)
            for gi in range(len(groups)):
                run2(post_group(gi), pre_group(gi + 1) if gi + 1 < len(groups) else None)

        if debug and "oT" in dbg:
            dt_ = k.alloc("dbgt", NH * L)
            k.cp(dt_.f(), oT.h(), [oT.buf], [dt_.buf])
            k.dma(dbg["oT"], dt_.f(), [dt_.buf], [])

        dn_regs = [small, prm, wab, cw, wp[0], qkv, szr, S32, Sbf] + cinL + caccL + csqL + [crnL[0]] + dgL
        for T_ in UT + PT + HT[0] + HT[1]:
            dn_regs += list(T_.values())
        k.kill(dn_regs)
        k.top = mark_dn

        k.phase(3)
        def layer_norm(xin, xbuf, gam, bet, pbufs, outs, obufs, stats):
            st6 = stats.f(0, 12).rearrange("p (a b) -> p a b", b=6)
            k.generic(DVE, lambda e: e.bn_stats(out=st6[:, 0, :], in_=xin[:, 0:512]), [xbuf], [stats.buf])
            k.generic(DVE, lambda e: e.bn_stats(out=st6[:, 1, :], in_=xin[:, 512:1024]), [xbuf], [stats.buf])
            k.generic(DVE, lambda e: e.bn_aggr(out=stats.f(12, 14), in_=stats.f(0, 12)), [stats.buf], [stats.buf])
            k.rsqrt(stats.f(14, 15), stats.f(13, 14), LN_EPS, [stats.buf], [stats.buf])
            k.ts(xin, xin, stats.f(12, 13), stats.f(14, 15), ALU.subtract, ALU.mult, [xbuf, stats.buf], [xbuf])
            k.tt(xin, xin, gam, ALU.mult, [xbuf] + pbufs, [xbuf])
            for o, ob in zip(outs, obufs):
                k.tt(o, xin, bet, ALU.add, [xbuf] + pbufs, [ob])

        lnp = k.alloc("lnp", 2 * D + 1024)
        SK = os.environ.get('KSKIP', '')
        if 'b' not in SK:
            bcast_load(lnp, sgu_ln_g, D, 0)
            bcast_load(lnp, sgu_ln_b, D, D)
            bcast_load(lnp, spb, 1024, 2 * D)
        wsp = k.alloc("wsp", 8 * 128)
        wspv = wsp.f().rearrange("p (g t) -> p g t", t=128)
        if 'a' not in SK:
            k.dma(wspv, spwT.rearrange("g s t -> s g t"), (), [wsp.buf])
        wspb = k.alloc("wspb", 8 * 64)
        wspbv = wspb.h().rearrange("p (g t) -> p g t", t=128)
        if 'c' not in SK:
            k.tt(wspbv, wspv, CF("UI").unsqueeze(1).to_broadcast([128, 8, 128]), ALU.mult, [wsp.buf, cf.buf], [wspb.buf])
        gT = k.alloc("gT", KC * L // 2)
        gTv = gT.h().rearrange("p (c t) -> p c t", t=L)
        wblk = [k.alloc(f"wblk{j}", KC * 512 // 2) for j in range(2)]
        vtmp = k.alloc("vtmp", D)
        stt_ = k.alloc("stats", 16)
        mark_vn = k.top
        vn = k.alloc("vn", NT * D // 2)
        vnv = vn.h().rearrange("p (t n) -> p t n", n=D)
        k.phase(3.1)
        nblk = 0
        for half in range(2):
            wr = wblk[nblk % 2]
            nblk += 1
            wvv = wr.h().rearrange("p (c n) -> p c n", n=512)
            c0 = C_UV + D + half * 512
            k.dma(wvv, w_in[:, c0:c0 + 512].rearrange("(c p) n -> p c n", p=128), (), [wr.buf], q=POOL)
            for i in range(NT):
                pb, pbuf = k.bank()
                for c in range(KC):
                    k.mm(pb, xTv[:, c, i * 128:(i + 1) * 128], wvv[:, c, :], c == 0, c == KC - 1, [xT.buf, wr.buf], [pbuf])
                k.act(vnv[:, i, half * 512:(half + 1) * 512], pb, AF.Gelu, [pbuf], [vn.buf])
        k.phase(3.2)
        for i in range(NT):
            k.act(vtmp.f(), vnv[:, i, :], AF.Copy, [vn.buf], [vtmp.buf])
            layer_norm(vtmp.f(), vtmp.buf, lnp.f(0, D), lnp.f(D, 2 * D), [lnp.buf], [vnv[:, i, :]], [vn.buf], stt_)
        k.phase(3.3)
        for half in range(2):
            wr = wblk[nblk % 2]
            nblk += 1
            wvv = wr.h().rearrange("p (c n) -> p c n", n=512)
            c0 = C_UV + half * 512
            k.dma(wvv, w_in[:, c0:c0 + 512].rearrange("(c p) n -> p c n", p=128), (), [wr.buf], q=POOL)
            for cc in range(4):
                for tb in range(4):
                    pb, pbuf = k.bank()
                    for c in range(KC):
                        k.mm(pb, wvv[:, c, cc * 128:(cc + 1) * 128], xTv[:, c, tb * 512:(tb + 1) * 512],
                             c == 0, c == KC - 1, [xT.buf, wr.buf], [pbuf])
                    k.act(gTv[:, half * 4 + cc, tb * 512:(tb + 1) * 512], pb, AF.Gelu, [pbuf], [gT.buf])
        k.phase(3.4)
        for i in range(NT):
            for gh in range(2):
                pb, pbuf = k.bank()
                for gq in range(4):
                    g = gh * 4 + gq
                    k.mm(pb[:, gq * 128:(gq + 1) * 128], vnv[:, i, g * 128:(g + 1) * 128], wspbv[:, g, :], True, True,
                         [vn.buf, wspb.buf], [pbuf])
                tmpv = vtmp.f(0, 512).rearrange("p (g t) -> p g t", t=128)
                k.tt(tmpv, pb.rearrange("p (g t) -> p g t", t=128),
                     lnp.f(2 * D + gh * 512, 2 * D + (gh + 1) * 512).rearrange("p (g t) -> p g t", t=128), ALU.add,
                     [pbuf, lnp.buf], [vtmp.buf])
                k.tt(gTv[:, gh * 4:(gh + 1) * 4, i * 128:(i + 1) * 128], gTv[:, gh * 4:(gh + 1) * 4, i * 128:(i + 1) * 128],
                     tmpv, ALU.mult, [gT.buf, vtmp.buf], [gT.buf])
        k.kill([vn, wblk[0], wblk[1], vtmp, stt_, lnp, wsp, wspb])
        k.top = mark_vn
        k.phase(4)
        yT = k.alloc("yT", KC * L // 2)
        yTv = yT.h().rearrange("p (c t) -> p c t", t=L)
        wm = [k.alloc(f"wm{j}", 4 * KC * 128 // 2) for j in range(2)]
        sg = [k.alloc(f"sg{j}", 512) for j in range(2)]
        for dc in range(KC):
            wr = wm[dc % 2]
            wmv = wr.h().rearrange("p (j c n) -> p j c n", j=4, n=128)
            srcs = (w_dn_out[:, dc * 128:(dc + 1) * 128], w_sgu_out[:, dc * 128:(dc + 1) * 128],
                    w_in[:, C_GDN + dc * 128:C_GDN + (dc + 1) * 128], w_in[:, C_GSGU + dc * 128:C_GSGU + (dc + 1) * 128])
            for j, s_ in enumerate(srcs):
                k.dma(wmv[:, j], s_.rearrange("(c p) n -> p c n", p=128), (), [wr.buf], q=POOL, acc=True)
            for tb in range(4):
                tsl = slice(tb * 512, (tb + 1) * 512)
                banks = [k.bank() for _ in range(4)]
                rhs_src = (oTv, gTv, xTv, xTv)
                rbufs = (oT.buf, gT.buf, xT.buf, xT.buf)
                for j in range(4):
                    pb, pbuf = banks[j]
                    for c in range(KC):
                        k.mm(pb, wmv[:, j, c, :], rhs_src[j][:, c, tsl], c == 0, c == KC - 1, [wr.buf, rbufs[j]], [pbuf])
                k.act(sg[0].f(), banks[2][0], AF.Sigmoid, [banks[2][1]], [sg[0].buf])
                k.act(sg[1].f(), banks[3][0], AF.Sigmoid, [banks[3][1]], [sg[1].buf])
                k.tt(sg[0].f(), sg[0].f(), banks[0][0], ALU.mult, [sg[0].buf, banks[0][1]], [sg[0].buf])
                k.tt(sg[1].f(), sg[1].f(), banks[1][0], ALU.mult, [sg[1].buf, banks[1][1]], [sg[1].buf])
                k.tt(yTv[:, dc, tsl], sg[0].f(), sg[1].f(), ALU.add, [sg[0].buf, sg[1].buf], [yT.buf])

        k.phase(5)
        k.kill([xT, oT, gT, wm[0], wm[1], sg[0], sg[1]])
        k.top = cb.off + cb.n
        comb = k.alloc("comb", NT * NE)
        combv = comb.f().rearrange("p (t e) -> p t e", e=NE)
        h1T = k.alloc("h1T", KC * L // 2)
        h1Tv = h1T.h().rearrange("p (c t) -> p c t", t=L)
        mark5 = k.top
        ln1p = k.alloc("ln1p", 2 * D)
        bcast_load(ln1p, ln1_g, D, 0)
        bcast_load(ln1p, ln1_b, D, D)
        wo = k.alloc("wo", KC * D // 2)
        wov = wo.h().rearrange("p (c n) -> p c n", n=D)
        k.dma(wov, w_out.rearrange("(c p) n -> p c n", p=128), (), [wo.buf], q=POOL)
        wrt = k.alloc("wrt", KC * 20 + 24)
        wrtv = wrt.f(0, KC * 20).rearrange("p (c n) -> p c n", n=20)
        k.dma(wrtv, w_rt.rearrange("(c p) n -> p c n", p=128), (), [wrt.buf])
        bcast_load(wrt, b_rt, 20, KC * 20)
        xres = [k.alloc(f"xres{j}", D) for j in range(2)]
        h1t = [k.alloc(f"h1t{j}", D) for j in range(2)]
        h1Tf = k.alloc("h1Tf", KC * 128)
        h1Tfv = h1Tf.f().rearrange("p (c t) -> p c t", t=128)
        stat1 = k.alloc("stat1", 16)
        rt = k.alloc("rt", 256)
        identf = CF("ident")
        for i in range(NT):
            xr, hb = xres[i % 2], h1t[i % 2]
            k.dma(xr.f(), x_d[i * 128:(i + 1) * 128, :], (), [xr.buf])
            for half in range(2):
                pb, pbuf = k.bank()
                for c in range(KC):
                    k.mm(pb, yTv[:, c, i * 128:(i + 1) * 128], wov[:, c, half * 512:(half + 1) * 512], c == 0, c == KC - 1,
                         [yT.buf, wo.buf], [pbuf])
                k.stt(hb.f(half * 512, (half + 1) * 512), xr.f(half * 512, (half + 1) * 512), ALPHA, pb, ALU.mult, ALU.add,
                      [xr.buf, pbuf], [hb.buf])
            layer_norm(hb.f(), hb.buf, ln1p.f(0, D), ln1p.f(D, 2 * D), [ln1p.buf], [hb.f()], [hb.buf], stat1)
            k.dma(h1_d[i * 128:(i + 1) * 128, :], hb.f(), [hb.buf], [h1bufs[i]])
            for c4 in range(2):
                pb, pbuf = k.bank()
                for cc in range(4):
                    c = c4 * 4 + cc
                    k.tr(pb[:, cc * 128:(cc + 1) * 128], hb.f(c * 128, (c + 1) * 128), identf, [hb.buf, cf.buf], [pbuf])
                k.act(h1Tv[:, c4 * 4:(c4 + 1) * 4, i * 128:(i + 1) * 128], pb.rearrange("p (c t) -> p c t", t=128), AF.Copy,
                      [pbuf], [h1T.buf])
                k.cp(h1Tfv[:, c4 * 4:(c4 + 1) * 4, :], pb.rearrange("p (c t) -> p c t", t=128), [pbuf], [h1Tf.buf])
            pb, pbuf = k.bank()
            for c in range(KC):
                k.mm(pb[:, 0:20], h1Tfv[:, c, :], wrtv[:, c, :], c == 0, c == KC - 1, [h1Tf.buf, wrt.buf], [pbuf])
            R_ = lambda a, b: rt.f(a, b)
            rb = [rt.buf]
            k.tt(R_(0, 20), pb[:, 0:20], wrt.f(KC * 20, KC * 20 + 20), ALU.add, [pbuf, wrt.buf], rb)
            k.generic(DVE, lambda e: e.reduce_max(out=R_(20, 21), in_=R_(0, 4), axis=AX.X), rb, rb)
            k.ts(R_(24, 28), R_(0, 4), R_(20, 21), None, ALU.is_equal, None, rb, rb)
            k.ts(R_(28, 32), R_(0, 4), R_(20, 21), None, ALU.subtract, None, rb, rb)
            k.memset(R_(21, 22), 0.0, rb)
            k.act(R_(28, 32), R_(28, 32), AF.Exp, rb, rb, accum=R_(21, 22))
            k.generic(DVE, lambda e: e.reciprocal(out=R_(22, 23), in_=R_(21, 22)), rb, rb)
            el = R_(4, 20).rearrange("p (g j) -> p g j", j=4)
            k.tt(R_(32, 48).rearrange("p (g j) -> p g j", j=4), el, R_(24, 28).unsqueeze(2).to_broadcast([128, 4, 4]),
                 ALU.mult, rb, rb)
            k.generic(DVE, lambda e: e.reduce_sum(out=R_(48, 52), in_=R_(32, 48).rearrange("p (g j) -> p j g", j=4),
                                                  axis=AX.X), rb, rb)
            k.generic(DVE, lambda e: e.reduce_max(out=R_(52, 53), in_=R_(48, 52), axis=AX.X), rb, rb)
            k.ts(R_(56, 60), R_(48, 52), R_(52, 53), None, ALU.is_equal, None, rb, rb)
            k.stt(R_(60, 64), R_(56, 60), -1e30, R_(48, 52), ALU.mult, ALU.add, rb, rb)
            k.generic(DVE, lambda e: e.reduce_max(out=R_(53, 54), in_=R_(60, 64), axis=AX.X), rb, rb)
            k.ts(R_(64, 68), R_(60, 64), R_(53, 54), None, ALU.is_equal, None, rb, rb)
            k.tt(R_(54, 55), R_(53, 54), R_(52, 53), ALU.subtract, rb, rb)
            k.act(R_(54, 55), R_(54, 55), AF.Exp, rb, rb)
            k.ts(R_(54, 55), R_(54, 55), 1.0, None, ALU.add, None, rb, rb)
            k.generic(DVE, lambda e: e.reciprocal(out=R_(68, 69), in_=R_(54, 55)), rb, rb)
            k.ts(R_(69, 70), R_(68, 69), -1.0, 1.0, ALU.mult, ALU.add, rb, rb)
            k.tt(R_(68, 70), R_(68, 70), R_(22, 23).to_broadcast([128, 2]), ALU.mult, rb, rb)
            k.ts(R_(72, 76), R_(56, 60), R_(68, 69), None, ALU.mult, None, rb, rb)
            k.stt(R_(72, 76), R_(64, 68), R_(69, 70), R_(72, 76), ALU.mult, ALU.add, rb, rb)
            k.tt(combv[:, i, :].rearrange("p (g j) -> p g j", j=4), R_(24, 28).unsqueeze(2).to_broadcast([128, 4, 4]),
                 R_(72, 76).unsqueeze(1).to_broadcast([128, 4, 4]), ALU.mult, rb, [comb.buf])

        k.phase(6)
        k.kill([yT, ln1p, wo, wrt, xres[0], xres[1], h1t[0], h1t[1], h1Tf, stat1, rt])
        k.top = mark5
        yacc = k.alloc("yacc", NT * D)
        yaccv = yacc.f().rearrange("p (t n) -> p t n", n=D)
        ew = [k.alloc(f"ew{j}", (2 * KC * FF + 2 * D) // 2) for j in range(2)]
        hT = [k.alloc(f"hT{j}", 2 * 512 // 2) for j in range(2)]
        sgl = [k.alloc(f"sgl{j}", 512) for j in range(2)]

        ln2p = k.alloc("ln2p", 2 * D)
        bcast_load(ln2p, ln2_g, D, 0)
        bcast_load(ln2p, ln2_b, D, D)
        hres = [k.alloc(f"hres{j}", D) for j in range(2)]
        stat2 = k.alloc("stat2", 16)
        yb = [Buf(f"yacc{i}") for i in range(NT)]
        for b_ in yb:
            b_.readers = dict(yacc.buf.readers)

        def final_tile(i):
            hr = hres[i % 2]
            k.dma(hr.f(), h1_d[i * 128:(i + 1) * 128, :], [h1bufs[i]], [hr.buf])
            k.stt(hr.f(), hr.f(), ALPHA, yaccv[:, i, :], ALU.mult, ALU.add, [hr.buf, yb[i]], [hr.buf])
            layer_norm(hr.f(), hr.buf, ln2p.f(0, D), ln2p.f(D, 2 * D), [ln2p.buf], [hr.f()], [hr.buf], stat2)
            k.dma(out_d[i * 128:(i + 1) * 128, :], hr.f(), [hr.buf], [])

        def load_expert(e, wr):
            v = wr.h()
            g = v[:, 0:KC * FF].rearrange("p (c n) -> p c n", n=FF)
            u = v[:, KC * FF:2 * KC * FF].rearrange("p (c n) -> p c n", n=FF)
            d = v[:, 2 * KC * FF:2 * KC * FF + 2 * D].rearrange("p (c n) -> p c n", n=D)
            k.dma(g, ew_gate[e].rearrange("(c p) n -> p c n", p=128), (), [wr.buf], q=POOL, acc=True)
            k.dma(u, ew_up[e].rearrange("(c p) n -> p c n", p=128), (), [wr.buf], q=POOL, acc=True)
            k.dma(d, ew_down[e].rearrange("(c p) n -> p c n", p=128), (), [wr.buf], q=POOL, acc=True)
            return g, u, d

        nxt = load_expert(0, ew[0])
        for e in range(NE):
            g_, u_, d_ = nxt
            wr = ew[e % 2]
            if e + 1 < NE:
                nxt = load_expert(e + 1, ew[(e + 1) % 2])
            for tb in range(4):
                tsl = slice(tb * 512, (tb + 1) * 512)
                hr = hT[tb % 2]
                hv = hr.h().rearrange("p (c t) -> p c t", t=512)
                for fc in range(2):
                    pg, pgb = k.bank()
                    pu, pub = k.bank()
                    for c in range(KC):
                        k.mm(pg, g_[:, c, fc * 128:(fc + 1) * 128], h1Tv[:, c, tsl], c == 0, c == KC - 1, [wr.buf, h1T.buf], [pgb])
                    for c in range(KC):
                        k.mm(pu, u_[:, c, fc * 128:(fc + 1) * 128], h1Tv[:, c, tsl], c == 0, c == KC - 1, [wr.buf, h1T.buf], [pub])
                    sl = sgl[fc]
                    k.act(sl.f(), pg, AF.Silu, [pgb], [sl.buf])
                    k.tt(hv[:, fc, :], sl.f(), pu, ALU.mult, [sl.buf, pub], [hr.buf])
                for ti in range(4):
                    i = tb * 4 + ti
                    for half in range(2):
                        pb, pbuf = k.bank()
                        for fc in range(2):
                            k.mm(pb, hv[:, fc, ti * 128:(ti + 1) * 128], d_[:, fc, half * 512:(half + 1) * 512], fc == 0, fc == 1,
                                 [hr.buf, wr.buf], [pbuf])
                        dst = yaccv[:, i, half * 512:(half + 1) * 512]
                        if e == 0:
                            k.ts(dst, pb, combv[:, i, e:e + 1], None, ALU.mult, None, [pbuf, comb.buf], [yb[i]])
                        else:
                            k.stt(dst, pb, combv[:, i, e:e + 1], dst, ALU.mult, ALU.add, [pbuf, comb.buf, yb[i]], [yb[i]])
                    if e == NE - 1:
                        final_tile(i)

        k.enabled = True
        for nm in [x for x in os.environ.get('KDUMP', '').split(',') if x]:
            rg = k.registry[nm]
            dd = nc.dram_tensor('dump_' + nm, [128, rg.n], F32, kind='ExternalOutput').ap()
            k.dma(dd, rg.f(), [rg.buf], [])
        k.B.emit()
        if os.environ.get('KVERBOSE'):
            print('maxwait', {kk: v for kk, v in k.B.maxwait.items() if kk[0] == 'e'})
    return nc


_NC_CACHE = {}


def _prep_inputs(inputs, b):
    f = lambda a: np.ascontiguousarray(np.asarray(a, dtype=np.float32))
    x = np.asarray(inputs["x"], dtype=np.float32)
    m = {}
    m["x"] = f(x[b])
    m["xT"] = f(x[b].T)
    m["w_in"] = f(inputs["w_in"][0])
    cwv = np.asarray(inputs["conv_w"], dtype=np.float32)[0]
    m["convT"] = f(cwv.T.reshape(24, 128, 4).transpose(1, 0, 2).reshape(128, 96))
    m["a_log"] = f(inputs["a_log"]).reshape(1, NH)
    m["dt_bias"] = f(inputs["dt_bias"]).reshape(1, NH)
    m["dn_norm_g"] = f(inputs["dn_norm_g"]).reshape(1, 128)
    m["w_dn_out"] = f(inputs["w_dn_out"][0])
    m["sgu_ln_g"] = f(inputs["sgu_ln_g"]).reshape(1, D)
    m["sgu_ln_b"] = f(inputs["sgu_ln_b"]).reshape(1, D)
    m["spwT"] = f(np.asarray(inputs["spatial_w"], dtype=np.float32)[0].transpose(0, 2, 1))
    m["spb"] = f(inputs["spatial_b"]).reshape(1, 1024)
    m["w_sgu_out"] = f(inputs["w_sgu_out"][0])
    m["w_out"] = f(inputs["w_out"][0])
    m["ln1_g"] = f(inputs["ln1_g"]).reshape(1, D)
    m["ln1_b"] = f(inputs["ln1_b"]).reshape(1, D)
    m["w_rt"] = f(np.concatenate([np.asarray(inputs["router_group_w"], dtype=np.float32)[0],
                                  np.asarray(inputs["router_expert_w"], dtype=np.float32)[0]], axis=1))
    m["b_rt"] = f(np.concatenate([np.asarray(inputs["router_group_b"], dtype=np.float32)[0],
                                  np.asarray(inputs["router_expert_b"], dtype=np.float32)[0]], axis=0)).reshape(1, 20)
    m["ew_gate"] = f(inputs["expert_w_gate"][0])
    m["ew_up"] = f(inputs["expert_w_up"][0])
    m["ew_down"] = f(inputs["expert_w_down"][0])
    m["ln2_g"] = f(inputs["ln2_g"]).reshape(1, D)
    m["ln2_b"] = f(inputs["ln2_b"]).reshape(1, D)
    m["ctab"] = CARR
    return m


def kernel(**inputs):
    if "nc" not in _NC_CACHE:
        _NC_CACHE["nc"] = build_program()
    nc = _NC_CACHE["nc"]
    shared = None
    in_maps = []
    for b in range(8):
        m = _prep_inputs(inputs, b) if shared is None else dict(shared)
        if shared is None:
            shared = m
        else:
            x = np.asarray(inputs["x"], dtype=np.float32)
            m["x"] = np.ascontiguousarray(x[b])
            m["xT"] = np.ascontiguousarray(x[b].T)
        in_maps.append(m)
    res = run_bass_kernel_spmd(nc, in_maps, core_ids=list(range(8)))
    out = np.stack([np.asarray(r["out"], dtype=np.float32) for r in res.results], axis=0)
    return out
```
